# Optimizing a Trainium2 kernel written in Bass

```python
import jax, jax.numpy as jnp
from jax import lax
import numpy as np

D_MODEL = 1024
BATCH = 8
SEQ = 4096
DEPTH = 1

GRID_W = 64
CTX_LEN = 256

F_GROUP_W = 128
F_WIDTH = D_MODEL // 2
F_GROUPS = F_WIDTH // F_GROUP_W

D_INNER = (3 * D_MODEL) // 2
SSD_HEAD_DIM = 64
SSD_HEADS = D_INNER // SSD_HEAD_DIM
SSD_GROUPS = 4
D_STATE = 128
CONV_W = 3
CHUNK = 128
CONV_CH = D_INNER + 2 * SSD_GROUPS * D_STATE

N_BRANCH = 2
P_IN = F_WIDTH + D_INNER + CONV_CH + 2 * SSD_HEADS + N_BRANCH * D_MODEL

N_EXPERTS = 16
EC_FACTOR = 2
EXPERT_FF = D_MODEL

N_MOD = 6
RMS_EPS = 1e-6

kernel_name = "hybrid_fnet_ssd_ec_moe_dit_block"


def rms_norm(x, w):
    xf = x.astype(jnp.float32)
    y = xf * lax.rsqrt(jnp.mean(xf * xf, axis=-1, keepdims=True) + RMS_EPS)
    return (y * w.astype(jnp.float32)).astype(x.dtype)


def modulate(x, gain, shift, scale):
    return rms_norm(x, gain) * (1 + scale) + shift


def centred_dwconv(u, w, b, n_rows):
    bn, length, ch = u.shape
    row_len = length // n_rows
    r = u.reshape(bn, n_rows, row_len, ch)
    pad = CONV_W // 2
    rp = jnp.pad(r, ((0, 0), (0, 0), (pad, pad), (0, 0)))
    out = sum(rp[:, :, k:k + row_len] * w[k] for k in range(CONV_W)) + b
    return out.reshape(bn, length, ch)


def ssd_chunked(xh, dt, a, bm, cm, h0):
    bn, length, nh, hp = xh.shape
    g, n = bm.shape[2], bm.shape[3]
    hg = nh // g
    nc = length // CHUNK
    x = xh.reshape(bn, nc, CHUNK, g, hg, hp)
    dtc = dt.reshape(bn, nc, CHUNK, g, hg)
    bc = bm.reshape(bn, nc, CHUNK, g, n)
    cc = cm.reshape(bn, nc, CHUNK, g, n)
    acum = jnp.cumsum(dtc * a.reshape(g, hg), axis=2)
    xdt = x * dtc[..., None]
    seg = acum[:, :, :, None] - acum[:, :, None, :]
    lower = jnp.tril(jnp.ones((CHUNK, CHUNK), dtype=bool))[:, :, None, None]
    decay = jnp.where(lower, jnp.exp(jnp.where(lower, seg, 0.0)), 0.0)
    cb = jnp.einsum('bctgn,bcsgn->bctsg', cc, bc)
    y_diag = jnp.einsum('bctsg,bctsgh,bcsghp->bctghp', cb, decay, xdt)
    decay_end = jnp.exp(acum[:, :, -1:] - acum)
    states = jnp.einsum('bcsgn,bcsgh,bcsghp->bcghpn', bc, decay_end, xdt)
    chunk_decay = jnp.exp(acum[:, :, -1])

    def step(h, inp):
        dec, st = inp
        return dec[..., None, None] * h + st, h

    h_final, h_prev = lax.scan(step, h0, (jnp.moveaxis(chunk_decay, 1, 0), jnp.moveaxis(states, 1, 0)))
    h_prev = jnp.moveaxis(h_prev, 0, 1)
    y_off = jnp.einsum('bctgn,bctgh,bcghpn->bctghp', cc, jnp.exp(acum), h_prev)
    y = (y_diag + y_off).reshape(bn, length, nh, hp)
    return y, h_final


def bidir_ssd(xh, dt_raw, bm, cm, dt_bias, a_log, h0_f, h0_b):
    dt_f = jax.nn.softplus(dt_raw[:, :, 0] + dt_bias[0])
    dt_b = jax.nn.softplus(dt_raw[:, :, 1] + dt_bias[1])
    a_f = -jnp.exp(a_log[0].astype(jnp.float32))
    a_b = -jnp.exp(a_log[1].astype(jnp.float32))
    y_f, h_f = ssd_chunked(xh, dt_f, a_f, bm, cm, h0_f)
    flip = lambda t: jnp.flip(t, axis=1)
    y_b, h_b = ssd_chunked(flip(xh), flip(dt_b), a_b, flip(bm), flip(cm), h0_b)
    return y_f + flip(y_b), h_f, h_b


def token_mixer(h, n_rows, h0_f, h0_b, w_in, conv_w, conv_b, dt_bias, a_log, d_skip, ssd_norm,
                w_fourier, w_ssd_out, b_gate, w_out):
    bn, length, _ = h.shape
    proj = h @ w_in
    i1 = F_WIDTH
    i2 = i1 + D_INNER
    i3 = i2 + CONV_CH
    i4 = i3 + 2 * SSD_HEADS
    u_f, z, xbc, dt_raw, gate_logits = jnp.split(proj, [i1, i2, i3, i4], axis=-1)
    uf = u_f.reshape(bn, length, F_GROUPS, F_GROUP_W).astype(jnp.float32)
    f = jnp.fft.fft2(uf, axes=(1, 3), norm='ortho').real.astype(h.dtype).reshape(bn, length, F_WIDTH)
    f_branch = f @ w_fourier
    xbc = jax.nn.silu(centred_dwconv(xbc, conv_w, conv_b, n_rows))
    xs, bm, cm = jnp.split(xbc, [D_INNER, D_INNER + SSD_GROUPS * D_STATE], axis=-1)
    xh = xs.reshape(bn, length, SSD_HEADS, SSD_HEAD_DIM).astype(jnp.float32)
    bm = bm.reshape(bn, length, SSD_GROUPS, D_STATE).astype(jnp.float32)
    cm = cm.reshape(bn, length, SSD_GROUPS, D_STATE).astype(jnp.float32)
    dt_raw = dt_raw.reshape(bn, length, 2, SSD_HEADS).astype(jnp.float32)
    y, h_f, h_b = bidir_ssd(xh, dt_raw, bm, cm, dt_bias, a_log, h0_f, h0_b)
    y = y + d_skip[:, None].astype(jnp.float32) * xh
    y = y.reshape(bn, length, D_INNER).astype(h.dtype)
    y = rms_norm(y * jax.nn.silu(z), ssd_norm)
    s_branch = y @ w_ssd_out
    g = jax.nn.sigmoid(gate_logits.reshape(bn, length, N_BRANCH, D_MODEL) + b_gate)
    merged = g[:, :, 0] * f_branch + g[:, :, 1] * s_branch
    return merged @ w_out, h_f, h_b


def expert_choice_ffn(h, w_router, w_e_gate, w_e_up, w_e_down):
    bn, length, d = h.shape
    cap = EC_FACTOR * length // N_EXPERTS
    aff = jax.nn.softmax((h @ w_router).astype(jnp.float32), axis=-1)
    top_aff, top_idx = lax.top_k(jnp.swapaxes(aff, 1, 2), cap)
    xe = jax.vmap(lambda hb, ib: hb[ib])(h, top_idx)
    hid = jax.nn.silu(jnp.einsum('becd,edf->becf', xe, w_e_gate)) * jnp.einsum('becd,edf->becf', xe, w_e_up)
    ye = jnp.einsum('becf,efd->becd', hid, w_e_down) * top_aff[..., None].astype(h.dtype)
    return jax.vmap(lambda ib, yb: jnp.zeros((length, d), yb.dtype).at[ib.reshape(-1)].add(yb.reshape(-1, d)))(top_idx, ye)


def setup_inputs(seed: int = 0) -> dict:
    key = jax.random.key(seed)
    ks = jax.random.split(key, 32)
    nrm = lambda k, shape, s: jax.random.normal(k, shape, jnp.float32) * s
    L = DEPTH
    dt0 = jnp.exp(jax.random.uniform(ks[12], (L, 2, SSD_HEADS), jnp.float32, np.log(1e-3), np.log(1e-1)))
    return {
        'x': nrm(ks[0], (BATCH, SEQ, D_MODEL), 1.0),
        'c': nrm(ks[1], (BATCH, D_MODEL), 1.0),
        'ctx': nrm(ks[2], (BATCH, CTX_LEN, D_MODEL), 1.0),
        'c_ctx': nrm(ks[3], (D_MODEL,), 1.0),
        'w_mod': nrm(ks[4], (L, D_MODEL, N_MOD * D_MODEL), 0.5 * D_MODEL ** -0.5),
        'b_mod': nrm(ks[5], (L, N_MOD * D_MODEL), 0.02),
        'norm_mix_pre': 1.0 + nrm(ks[6], (L, D_MODEL), 0.05),
        'norm_mix_post': 1.0 + nrm(ks[7], (L, D_MODEL), 0.05),
        'norm_ffn_pre': 1.0 + nrm(ks[8], (L, D_MODEL), 0.05),
        'norm_ffn_post': 1.0 + nrm(ks[9], (L, D_MODEL), 0.05),
        'w_in': nrm(ks[10], (L, D_MODEL, P_IN), D_MODEL ** -0.5),
        'conv_w': nrm(ks[11], (L, CONV_W, CONV_CH), CONV_W ** -0.5),
        'conv_b': nrm(ks[13], (L, CONV_CH), 0.02),
        'dt_bias': dt0 + jnp.log(-jnp.expm1(-dt0)),
        'a_log': jnp.log(jax.random.uniform(ks[14], (L, 2, SSD_HEADS), jnp.float32, 1.0, 16.0)),
        'd_skip': 1.0 + nrm(ks[15], (L, SSD_HEADS), 0.1),
        'ssd_norm': 1.0 + nrm(ks[16], (L, D_INNER), 0.05),
        'w_fourier': nrm(ks[17], (L, F_WIDTH, D_MODEL), F_WIDTH ** -0.5),
        'w_ssd_out': nrm(ks[18], (L, D_INNER, D_MODEL), D_INNER ** -0.5),
        'b_gate': nrm(ks[19], (L, N_BRANCH, D_MODEL), 0.1),
        'w_out': nrm(ks[20], (L, D_MODEL, D_MODEL), D_MODEL ** -0.5),
        'w_router': nrm(ks[21], (L, D_MODEL, N_EXPERTS), D_MODEL ** -0.5),
        'w_e_gate': nrm(ks[22], (L, N_EXPERTS, D_MODEL, EXPERT_FF), D_MODEL ** -0.5),
        'w_e_up': nrm(ks[23], (L, N_EXPERTS, D_MODEL, EXPERT_FF), D_MODEL ** -0.5),
        'w_e_down': nrm(ks[24], (L, N_EXPERTS, EXPERT_FF, D_MODEL), EXPERT_FF ** -0.5),
    }


def reference(x, c, ctx, c_ctx, w_mod, b_mod, norm_mix_pre, norm_mix_post, norm_ffn_pre, norm_ffn_post,
              w_in, conv_w, conv_b, dt_bias, a_log, d_skip, ssd_norm, w_fourier, w_ssd_out, b_gate, w_out,
              w_router, w_e_gate, w_e_up, w_e_down):
    bn, length, d = x.shape
    rows = length // GRID_W
    hx, hc = x, ctx
    for l in range(DEPTH):
        mod_x = (jax.nn.silu(c) @ w_mod[l] + b_mod[l]).reshape(bn, N_MOD, 1, d)
        mod_c = (jax.nn.silu(c_ctx) @ w_mod[l] + b_mod[l]).reshape(N_MOD, d)
        sh1, sc1, g1, sh2, sc2, g2 = [mod_x[:, i] for i in range(N_MOD)]
        csh1, csc1, cg1, csh2, csc2, cg2 = [mod_c[i] for i in range(N_MOD)]
        mix_w = (w_in[l], conv_w[l], conv_b[l], dt_bias[l], a_log[l], d_skip[l], ssd_norm[l],
                 w_fourier[l], w_ssd_out[l], b_gate[l], w_out[l])
        h0 = jnp.zeros((bn, SSD_GROUPS, SSD_HEADS // SSD_GROUPS, SSD_HEAD_DIM, D_STATE), jnp.float32)
        mix_c, hf_c, hb_c = token_mixer(modulate(hc, norm_mix_pre[l], csh1, csc1), 1, h0, h0, *mix_w)
        mix_x, _, _ = token_mixer(modulate(hx, norm_mix_pre[l], sh1, sc1), rows, hf_c, hb_c, *mix_w)
        hx = hx + g1 * rms_norm(mix_x, norm_mix_post[l])
        ffn_x = expert_choice_ffn(modulate(hx, norm_ffn_pre[l], sh2, sc2), w_router[l], w_e_gate[l], w_e_up[l], w_e_down[l])
        hx = hx + g2 * rms_norm(ffn_x, norm_ffn_post[l])
        if l + 1 < DEPTH:
            hc = hc + cg1 * rms_norm(mix_c, norm_mix_post[l])
            ffn_c = expert_choice_ffn(modulate(hc, norm_ffn_pre[l], csh2, csc2), w_router[l], w_e_gate[l], w_e_up[l], w_e_down[l])
            hc = hc + cg2 * rms_norm(ffn_c, norm_ffn_post[l])
    return hx
```

```python
import contextlib
import math
import os
F_SLOT = os.environ.get('F_SLOT', '1') == '1'
F_IDD = os.environ.get('F_IDD', '1') == '1'
F_PROJ = os.environ.get('F_PROJ', '1') == '1'
F_SAME = os.environ.get('F_SAME', '1') == '1'
import numpy as np
import concourse.bass as bass
import concourse.mybir as mybir
from concourse.bass_utils import run_bass_kernel_spmd

F32 = mybir.dt.float32
BF16 = mybir.dt.bfloat16
I32 = mybir.dt.int32
AF = mybir.ActivationFunctionType
ALU = mybir.AluOpType
AX = mybir.AxisListType

D = 1024
KC = 8
GRID_W = 64
FW = 512
DI = 1536
NH = 24
NG = 4
HG = 6
HP = 64
NS = 128
NE = 16
O_Z, O_X, O_B, O_C, O_DT, O_GATE = 512, 2048, 3584, 4096, 4608, 4656
P_IN = 6704
EPS = 1e-6
RW = 544
BIG = 1.0e6


class Tok:
    __slots__ = ("w", "r")

    def __init__(self):
        self.w = None
        self.r = {}


class Sched:
    CE = ("pe", "act", "dve", "pool")
    QE = ("sp", "act", "pool")
    NPOOL = 8

    def __init__(self, nc):
        self.nc = nc
        self.prog = {e: [] for e in ("pe", "act", "dve", "pool", "sp")}
        self.cnt = {e: 0 for e in self.CE}
        self.seen = {e: {} for e in self.prog}
        self.dma_n = {q: 0 for q in self.QE}
        self.dma_val = {}
        self.stack = contextlib.ExitStack()
        self.sems = {}
        for e in self.CE:
            self.sems[e] = self.stack.enter_context(nc.semaphore("s_" + e))
        for q in self.QE:
            for i in range(self.NPOOL):
                k = ("dma", q, i)
                self.sems[k] = self.stack.enter_context(nc.semaphore("d_%s%d" % (q, i)))
                self.dma_val[k] = 0
        self._regs = {}

    def _wait(self, e, k, v):
        seen = self.seen[e]
        if seen.get(k, 0) >= v:
            return
        seen[k] = v
        self.prog[e].append(("wait", k, v))

    def _deps(self, e, reads, writes):
        deps = {}
        for t in reads:
            if t.w is not None:
                k, v = t.w
                if deps.get(k, 0) < v:
                    deps[k] = v
        for t in writes:
            if t.w is not None:
                k, v = t.w
                if deps.get(k, 0) < v:
                    deps[k] = v
            for k, v in t.r.items():
                if deps.get(k, 0) < v:
                    deps[k] = v
        for k, v in deps.items():
            if k == e and (e == "pe" or not F_SAME):
                continue
            self._wait(e, k, v)

    def op(self, e, fn, reads=(), writes=()):
        self._deps(e, reads, writes)
        self.cnt[e] += 1
        v = self.cnt[e]
        self.prog[e].append(("op", fn, e, 1))
        for t in writes:
            t.w = (e, v)
            t.r = {}
        for t in reads:
            if t.r.get(e, 0) < v:
                t.r[e] = v

    def dma(self, q, fn, reads=(), writes=()):
        i = self.dma_n[q] % self.NPOOL
        self.dma_n[q] += 1
        k = ("dma", q, i)
        prev = self.dma_val[k]
        if prev > 0:
            self._wait(q, k, prev)
        self._deps(q, reads, writes)
        v = prev + 16
        self.dma_val[k] = v
        self.prog[q].append(("op", fn, k, 16))
        for t in writes:
            t.w = (k, v)
            t.r = {}
        for t in reads:
            if t.r.get(k, 0) < v:
                t.r[k] = v

    def reg(self, engine, value):
        key = (id(engine), value)
        if key not in self._regs:
            self._regs[key] = engine.to_reg(value)
        return self._regs[key]

    def barrier(self):
        for e in self.prog:
            for k in self.CE:
                if k != e and self.cnt[k] > 0:
                    self._wait(e, k, self.cnt[k])
            for k, v in self.dma_val.items():
                if v > 0:
                    self._wait(e, k, v)

    def emit(self):
        nc = self.nc
        sems = self.sems
        prog = self.prog
        with nc.Block() as block:
            def run(e, engine):
                for it in prog[e]:
                    if it[0] == "wait":
                        engine.wait_ge(sems[it[1]], it[2])
                    else:
                        _, fn, k, inc = it
                        fn(engine).then_inc(sems[k], inc)

            @block.sync
            def _(engine):
                run("sp", engine)

            @block.scalar
            def _(engine):
                run("act", engine)

            @block.vector
            def _(engine):
                run("dve", engine)

            @block.gpsimd
            def _(engine):
                run("pool", engine)

            @block.tensor
            def _(engine):
                run("pe", engine)
        self.stack.close()


def build_program(L, CTX, debug=False, stop_after=None):
    NT = L // 128
    NB = L // 512
    NTC = CTX // 128
    H2 = L // 2
    NHC = H2 // 128
    PW = min(512, H2)
    NPC = H2 // PW
    CAP = 2 * L // NE
    JT = CAP // 128
    assert CAP % 128 == 0 and L % 512 == 0 and CTX <= 512 and CTX % 128 == 0

    nc = bass.Bass("TRN2", target_bir_lowering=False)
    S = Sched(nc)
    okind = "ExternalOutput" if debug else "Internal"

    def din(name, shape, dt=F32):
        return nc.dram_tensor(name, list(shape), dt, kind="ExternalInput").ap()

    def dscr(name, shape, dt=F32):
        return nc.dram_tensor(name, list(shape), dt, kind=okind).ap()

    x_d = din("x", [L, D])
    ctx_d = din("ctx", [CTX, D])
    ccol_d = din("ccol", [128, KC, 2])
    w_mod_d = din("w_mod", [D, 6 * D])
    bmod_col_d = din("bmod_col", [128, 16])
    bmod_row_d = din("bmod_row", [1, 6 * D])
    nmp_col_d = din("nmp_col", [128, KC])
    nmpost_d = din("nmpost", [1, D])
    nfpre_d = din("nfpre", [1, D])
    nfpost_d = din("nfpost", [1, D])
    w_in_d = din("w_in", [D, P_IN])
    convw_d = din("convw", [128, 20, 3])
    convb_d = din("convb", [128, 20])
    dtb_d = din("dtb", [1, 48])
    alog_d = din("alog", [1, 48])
    dskip_d = din("dskip", [1, NH])
    ssdn_d = din("ssdn", [1, DI])
    w_four_d = din("w_four", [FW, D])
    w_ssd_d = din("w_ssd", [DI, D])
    bgate_d = din("bgate", [128, 16])
    w_out_d = din("w_out", [D, D])
    w_r_d = din("w_r", [D, NE])
    weg_d = din("weg", [NE, D, D])
    weu_d = din("weu", [NE, D, D])
    wed_d = din("wed", [NE, D, D])
    ck_d = din("ck", [128, 128])
    sk_d = din("sk", [128, 128])
    cmat_d = din("cmat", [H2, H2])
    smat_d = din("smat", [H2, H2])
    sgn_d = din("sgn", [1, H2])
    out_d = nc.dram_tensor("out", [L, D], F32, kind="ExternalOutput").ap()

    hT_d = dscr("hT_d", [128, KC, L], BF16)
    hTc_d = dscr("hTc_d", [128, KC, CTX], BF16)
    y_d = dscr("y_d", [L, DI])
    sbr_d = dscr("sbr_d", [128, KC, L], BF16)
    hx_d = dscr("hx_d", [L, D])
    h2_d = dscr("h2_d", [L, 512])
    ffn_d = dscr("ffn_d", [L, D])
    xg_d = dscr("xg_d", [NE * CAP, RW])
    dbg_d = dscr("dbg_d", [128, 4096]) if debug else None

    def MM(out, lhsT, rhs, st, sp, r, w):
        S.op("pe", lambda e: e.matmul(out, lhsT=lhsT, rhs=rhs, start=st, stop=sp), reads=r, writes=w)

    def TR(out, in_, ident, r, w):
        S.op("pe", lambda e: e.transpose(out, in_=in_, identity=ident), reads=r, writes=w)

    def ACT(out, in_, func, r, w, **kw):
        S.op("act", lambda e: e.activation(out=out, in_=in_, func=func, **kw), reads=r, writes=w)

    def TT(eng, out, in0, in1, op, r, w):
        S.op(eng, lambda e: e.tensor_tensor(out=out, in0=in0, in1=in1, op=op), reads=r, writes=w)

    def TS(eng, out, in0, s1, s2, op0, op1, r, w):
        if s2 is None:
            S.op(eng, lambda e: e.tensor_scalar(out=out, in0=in0, scalar1=s1, scalar2=None, op0=op0), reads=r, writes=w)
        else:
            S.op(eng, lambda e: e.tensor_scalar(out=out, in0=in0, scalar1=s1, scalar2=s2, op0=op0, op1=op1), reads=r, writes=w)

    def STT(eng, out, in0, scalar, in1, op0, op1, r, w):
        S.op(eng, lambda e: e.scalar_tensor_tensor(out=out, in0=in0, scalar=scalar, in1=in1, op0=op0, op1=op1), reads=r, writes=w)

    def CP(eng, out, in_, r, w):
        if eng == "act":
            ACT(out, in_, AF.Copy, r, w)
        else:
            S.op(eng, lambda e: e.tensor_copy(out=out, in_=in_), reads=r, writes=w)

    def RED(eng, out, in_, op, r, w):
        S.op(eng, lambda e: e.tensor_reduce(out=out, in_=in_, axis=AX.X, op=op), reads=r, writes=w)

    def MSET(eng, ap, val, w):
        S.op(eng, lambda e: e.memset(ap, val), writes=w)

    def RECIP(out, in_, r, w):
        S.op("dve", lambda e: e.reciprocal(out=out, in_=in_), reads=r, writes=w)

    def TSS(out, in_, scalar, op, r, w):
        S.op("dve", lambda e: e.tensor_single_scalar(out=out, in_=in_, scalar=scalar, op=op), reads=r, writes=w)

    def DMA(q, out, in_, r, w, **kw):
        S.dma(q, lambda e: e.dma_start(out=out, in_=in_, **kw), reads=r, writes=w)

    def wview(w_ap, c0, c1):
        return w_ap.rearrange("(kc p) n -> p kc n", p=128)[:, :, c0:c1]

    top = contextlib.ExitStack()

    uniq = [0]

    def SB(stack, name, shape, dt=F32):
        uniq[0] += 1
        return stack.enter_context(nc.sbuf_tensor("sb%d_%s" % (uniq[0], name), list(shape), dt))

    def PS(stack, name, shape, dt=F32):
        uniq[0] += 1
        return stack.enter_context(nc.psum_tensor("ps%d_%s" % (uniq[0], name), list(shape), dt))

    ident_f = SB(top, "ident_f", [128, 128]); t_const = Tok()
    ident_b = SB(top, "ident_b", [128, 128], BF16)
    ones_b = SB(top, "ones_b", [128, 128], BF16)
    ones_f = SB(top, "ones_f", [128, 128])
    ltri_b = SB(top, "ltri_b", [128, 128], BF16)
    utri_b = SB(top, "utri_b", [128, 128], BF16)
    slt_f = SB(top, "slt_f", [128, 128])
    mnegf_b = SB(top, "mnegf_b", [128, 128], BF16)
    mnegb_b = SB(top, "mnegb_b", [128, 128], BF16)
    tmpc = SB(top, "tmpc", [128, 128])
    one_col = SB(top, "one_col", [128, 1])

    def tri(dst, base_val, cm, step, cmp, fill):
        MSET("pool", tmpc[:], base_val, [t_const])
        S.op("pool", lambda e, o_=tmpc[:]: e.affine_select(out=o_, in_=o_, pattern=[[step, 128]], compare_op=cmp,
                                               fill=fill, base=0, channel_multiplier=cm), reads=[t_const], writes=[t_const])
        CP("pool", dst[:], tmpc[:], [t_const], [t_const])

    tri(ident_f, 1.0, 1, -1, ALU.is_equal, 0.0)
    tri(ident_b, 1.0, 1, -1, ALU.is_equal, 0.0)
    tri(ltri_b, 1.0, -1, 1, ALU.is_ge, 0.0)
    tri(utri_b, 1.0, 1, -1, ALU.is_ge, 0.0)
    tri(slt_f, 1.0, -1, 1, ALU.is_gt, 0.0)
    tri(mnegf_b, 0.0, -1, 1, ALU.is_ge, -1.0e4)
    tri(mnegb_b, 0.0, 1, -1, ALU.is_ge, -1.0e4)
    MSET("pool", ones_b[:], 1.0, [t_const])
    MSET("pool", ones_f[:], 1.0, [t_const])
    MSET("pool", one_col[:], 1.0, [t_const])
    C = [t_const]

    a1x = SB(top, "a1x", [128, KC]); b1x = SB(top, "b1x", [128, KC])
    a1c = SB(top, "a1c", [128, KC]); b1c = SB(top, "b1c", [128, KC])
    g1row = SB(top, "g1row", [128, D]); a2row = SB(top, "a2row", [128, D])
    b2row = SB(top, "b2row", [128, D]); g2row = SB(top, "g2row", [128, D])
    t_mod = Tok()
    stackG = contextlib.ExitStack()
    dt_x = SB(stackG, "dt_x", [128, NT, 48]); da_x = SB(stackG, "da_x", [128, NT, 48])
    dt_c = SB(stackG, "dt_c", [128, NTC, 48]); da_c = SB(stackG, "da_c", [128, NTC, 48])
    t_dt = Tok()
    sc_state = SB(stackG, "sc_state", [128, 2 * NG, 384]); t_scs = [Tok() for _ in range(2 * NG)]
    dskip_bc = SB(stackG, "dskip_bc", [128, NH]); t_dsk = Tok()
    DMA("sp", dskip_bc[:], dskip_d.to_broadcast([128, NH]), [], [t_dsk])

    with contextlib.ExitStack() as ph:
        sc_col = SB(ph, "sc_col", [128, KC, 2]); t_sc = Tok()
        bmc = SB(ph, "bmc", [128, 16]); nmp = SB(ph, "nmp", [128, KC]); t_sm = Tok()
        wm = [SB(ph, "wm%d" % i, [128, KC, D]) for i in range(2)]; t_wm = [Tok(), Tok()]
        modcol = SB(ph, "modcol", [128, 16, 2]); t_mc = Tok()
        rowtmp = SB(ph, "rowtmp", [128, D]); t_rt = Tok()
        nrow = SB(ph, "nrow", [128, D]); t_nr = Tok()
        pcol = PS(ph, "pcol", [128, 32]); t_pcol = Tok()
        prow = [PS(ph, "prow%d" % i, [128, 512]) for i in range(2)]; t_prow = [Tok(), Tok()]
        DMA("sp", sc_col[:], ccol_d, [], [t_sc])
        DMA("sp", bmc[:], bmod_col_d, [], [t_sm])
        DMA("sp", nmp[:], nmp_col_d, [], [t_sm])
        ACT(sc_col[:], sc_col[:], AF.Silu, [t_sc], [t_sc])
        for m in range(6):
            b = m % 2
            DMA("sp" if m % 2 == 0 else "act", wm[b][:], wview(w_mod_d, m * D, (m + 1) * D), [], [t_wm[b]])
            if m < 2:
                for dc in range(8):
                    for kc in range(KC):
                        MM(pcol[:, (m * 8 + dc) * 2:(m * 8 + dc) * 2 + 2], wm[b][:, kc, dc * 128:(dc + 1) * 128],
                           sc_col[:, kc, :], kc == 0, kc == KC - 1, [t_wm[b], t_sc], [t_pcol])
                if m == 1:
                    TT("dve", modcol[:], pcol[:].rearrange("p (a b) -> p a b", b=2),
                       bmc[:].unsqueeze(2).to_broadcast([128, 16, 2]), ALU.add, [t_pcol, t_sm], [t_mc])
            else:
                dst = {2: g1row, 3: b2row, 4: a2row, 5: g2row}[m]
                DMA("sp", rowtmp[:], bmod_row_d[:, m * D:(m + 1) * D].to_broadcast([128, D]), [], [t_rt])
                for hf in range(2):
                    for kc in range(KC):
                        MM(prow[hf][:], sc_col[:, kc, 0:1].to_broadcast([128, 128]), wm[b][:, kc, hf * 512:(hf + 1) * 512],
                           kc == 0, kc == KC - 1, [t_wm[b], t_sc], [t_prow[hf]])
                    TT("dve", dst[:, hf * 512:(hf + 1) * 512], prow[hf][:], rowtmp[:, hf * 512:(hf + 1) * 512], ALU.add,
                       [t_prow[hf], t_rt], [t_mod])
                if m == 2:
                    DMA("act", nrow[:], nmpost_d.to_broadcast([128, D]), [], [t_nr])
                    TT("dve", g1row[:], g1row[:], nrow[:], ALU.mult, [t_nr, t_mod], [t_mod])
                elif m == 4:
                    DMA("act", nrow[:], nfpre_d.to_broadcast([128, D]), [], [t_nr])
                    STT("dve", a2row[:], a2row[:], 1.0, nrow[:], ALU.add, ALU.mult, [t_nr, t_mod], [t_mod])
                elif m == 5:
                    DMA("act", nrow[:], nfpost_d.to_broadcast([128, D]), [], [t_nr])
                    TT("dve", g2row[:], g2row[:], nrow[:], ALU.mult, [t_nr, t_mod], [t_mod])
        STT("dve", a1x[:], modcol[:, 8:16, 0], 1.0, nmp[:], ALU.add, ALU.mult, [t_mc, t_sm], [t_mod])
        STT("dve", a1c[:], modcol[:, 8:16, 1], 1.0, nmp[:], ALU.add, ALU.mult, [t_mc, t_sm], [t_mod])
        CP("dve", b1x[:], modcol[:, 0:8, 0], [t_mc], [t_mod])
        CP("dve", b1c[:], modcol[:, 0:8, 1], [t_mc], [t_mod])
        S.barrier()
    if stop_after == "M":
        if debug:
            mod_dbg = nc.dram_tensor("mod_dbg", [128, 4, D], F32, kind="ExternalOutput").ap()
            for qi, tt_ in enumerate((g1row, a2row, b2row, g2row)):
                DMA("sp", mod_dbg[:, qi, :], tt_[:], [t_mod], [])
            col_dbg = nc.dram_tensor("col_dbg", [128, 4, KC], F32, kind="ExternalOutput").ap()
            for qi, tt_ in enumerate((a1x, b1x, a1c, b1c)):
                DMA("sp", col_dbg[:, qi, :], tt_[:], [t_mod], [])
        return finish(nc, S, top, out_d, dbg=None)

    with contextlib.ExitStack() as ph:
        xt = [SB(ph, "xt%d" % i, [128, D]) for i in range(2)]; t_xt = [Tok(), Tok()]
        junk = SB(ph, "junk", [128, D], BF16); t_junk = Tok()
        st4 = [SB(ph, "st4%d" % i, [128, 4]) for i in range(2)]; t_st = [Tok(), Tok()]
        xn = [SB(ph, "xn%d" % i, [128, D], BF16) for i in range(2)]; t_xn = [Tok(), Tok()]
        tmpf = SB(ph, "tmpf", [128, KC, 128]); t_tmpf = Tok()
        hTt = [SB(ph, "hTt%d" % i, [128, KC, 128], BF16) for i in range(2)]; t_hTt = [Tok(), Tok()]
        wdt = SB(ph, "wdt", [128, KC, 48], BF16); t_wdt = Tok()
        dtb_bc = SB(ph, "dtb_bc", [128, 48]); aneg_bc = SB(ph, "aneg_bc", [128, 48]); t_ab = Tok()
        pT = [PS(ph, "pT%d" % i, [128, D], BF16) for i in range(2)]; t_pT = [Tok(), Tok()]
        pdt = [PS(ph, "pdt%d" % i, [128, 64]) for i in range(2)]; t_pdt = [Tok(), Tok()]
        DMA("pool", wdt[:], wview(w_in_d, O_DT, O_DT + 48), [], [t_wdt])
        DMA("sp", dtb_bc[:], dtb_d.to_broadcast([128, 48]), [], [t_ab])
        DMA("sp", aneg_bc[:], alog_d.to_broadcast([128, 48]), [], [t_ab])
        ACT(aneg_bc[:], aneg_bc[:], AF.Exp, [t_ab], [t_ab])
        TS("dve", aneg_bc[:], aneg_bc[:], -1.0, None, ALU.mult, None, [t_ab], [t_ab])

        def norm_T(src, ntile, acol, bcol, dst, dtraw):
            for i in range(ntile):
                b = i % 2
                DMA("sp", xt[b][:], src[i * 128:(i + 1) * 128, :], [], [t_xt[b]])
                ACT(junk[:], xt[b][:], AF.Square, [t_xt[b]], [t_junk, t_st[b]], accum_out=st4[b][:, 0:1])
                TS("dve", st4[b][:, 1:2], st4[b][:, 0:1], 1.0 / D, EPS, ALU.mult, ALU.add, [t_st[b]], [t_st[b]])
                ACT(st4[b][:, 2:3], st4[b][:, 1:2], AF.Sqrt, [t_st[b]], [t_st[b]])
                RECIP(st4[b][:, 3:4], st4[b][:, 2:3], [t_st[b]], [t_st[b]])
                ACT(xn[b][:], xt[b][:], AF.Identity, [t_xt[b], t_st[b]], [t_xn[b]], scale=st4[b][:, 3:4])
                for kc in range(KC):
                    TR(pT[b][:, kc * 128:(kc + 1) * 128], xn[b][:, kc * 128:(kc + 1) * 128], ident_b[:], [t_xn[b]] + C, [t_pT[b]])
                TT("dve", tmpf[:], pT[b][:].rearrange("p (k t) -> p k t", k=KC), acol[:].unsqueeze(2).to_broadcast([128, KC, 128]),
                   ALU.mult, [t_pT[b], t_mod], [t_tmpf])
                TT("dve", hTt[b][:], tmpf[:], bcol[:].unsqueeze(2).to_broadcast([128, KC, 128]), ALU.add, [t_tmpf, t_mod], [t_hTt[b]])
                DMA("act", dst[:, :, i * 128:(i + 1) * 128], hTt[b][:], [t_hTt[b]], [])
                for kc in range(KC):
                    MM(pdt[b][:, 0:48], hTt[b][:, kc, :], wdt[:, kc, :], kc == 0, kc == KC - 1, [t_hTt[b], t_wdt], [t_pdt[b]])
                CP("act", dtraw[:, i, :], pdt[b][:, 0:48], [t_pdt[b]], [t_dt])

        def dt_post(dtt, dat, ntile, stack):
            va = SB(stack, "va%d" % ntile, [128, ntile, 48]); vb = SB(stack, "vb%d" % ntile, [128, ntile, 48]); t_v = Tok()
            TT("dve", dtt[:], dtt[:], dtb_bc[:].unsqueeze(1).to_broadcast([128, ntile, 48]), ALU.add, [t_dt, t_ab], [t_dt])
            TS("dve", va[:], dtt[:], 30.0, None, ALU.min, None, [t_dt], [t_v])
            ACT(va[:], va[:], AF.Exp, [t_v], [t_v])
            TS("dve", va[:], va[:], 1.0, None, ALU.add, None, [t_v], [t_v])
            ACT(vb[:], va[:], AF.Ln, [t_v], [t_v])
            TT("dve", dtt[:], dtt[:], vb[:], ALU.max, [t_dt, t_v], [t_dt])
            TT("dve", dat[:], dtt[:], aneg_bc[:].unsqueeze(1).to_broadcast([128, ntile, 48]), ALU.mult, [t_dt, t_ab], [t_dt])

        norm_T(ctx_d, NTC, a1c, b1c, hTc_d, dt_c)
        if stop_after == "N1":
            return finish(nc, S, top, out_d, dbg=None)
        dt_post(dt_c, da_c, NTC, ph)
        if stop_after == "N2":
            return finish(nc, S, top, out_d, dbg=None)
        norm_T(x_d, NT, a1x, b1x, hT_d, dt_x)
        dt_post(dt_x, da_x, NT, ph)
        S.barrier()
    if stop_after == "N":
        return finish(nc, S, top, out_d, dbg=None)

    zero_y = None
    with contextlib.ExitStack() as ph:
        wg = SB(ph, "wg", [128, KC, 640], BF16); t_wg = Tok()
        cw = SB(ph, "cw", [128, 20, 3]); cb = SB(ph, "cb", [128, 20]); t_cw = Tok()
        DMA("sp", cw[:], convw_d, [], [t_cw])
        DMA("sp", cb[:], convb_d, [], [t_cw])
        hTb = [SB(ph, "hTb%d" % i, [128, KC, 512], BF16) for i in range(2)]; t_hTb = [Tok(), Tok()]
        acc = [SB(ph, "acc%d" % i, [128, 512]) for i in range(2)]; t_acc = [Tok(), Tok()]
        xTb = [SB(ph, "xTb%d" % i, [128, 512], BF16) for i in range(2)]; t_xTb = [Tok(), Tok()]
        x_tm = SB(ph, "x_tm", [128, NT, 384], BF16); t_xtm = [Tok() for _ in range(NT)]
        b_tm = SB(ph, "b_tm", [128, NT, 128], BF16); t_btm = [Tok() for _ in range(NT)]
        bT = SB(ph, "bT", [128, L], BF16); t_bT = [Tok() for _ in range(NB)]
        cT = SB(ph, "cT", [128, L], BF16); t_cT = [Tok() for _ in range(NB)]
        identD = SB(ph, "identD", [128, NH, 128], BF16); t_idD = Tok()
        for hh in range(NH):
            TS("dve", identD[:, hh, :], ident_f[:], dskip_bc[:, hh:hh + 1], None, ALU.mult, None, [t_dsk] + C, [t_idD])

        class DirBufs:
            pass
        dirs = []
        for dd in range(2):
            o = DirBufs()
            sfx = "_d%d" % dd
            o.dahi = SB(ph, "dahi" + sfx, [128, NT, HG], BF16); o.dalo = SB(ph, "dalo" + sfx, [128, NT, HG], BF16); o.t_da = Tok()
            o.datmp = SB(ph, "datmp" + sfx, [128, NT, HG])
            o.nacum = SB(ph, "nacum" + sfx, [128, NT, HG]); o.eacum = SB(ph, "eacum" + sfx, [128, NT, HG])
            o.wst = SB(ph, "wst" + sfx, [128, NT, HG]); o.cdec = SB(ph, "cdec" + sfx, [128, NT, HG]); o.t_hd = Tok()
            o.xdt = [SB(ph, "xdt%d" % i + sfx, [128, 384], BF16) for i in range(2)]; o.t_xdt = [Tok(), Tok()]
            o.xw = [SB(ph, "xw%d" % i + sfx, [128, 384], BF16) for i in range(2)]; o.t_xw = [Tok(), Tok()]
            o.cbT = [SB(ph, "cbT%d" % i + sfx, [128, 128]) for i in range(2)]; o.t_cbT = [Tok(), Tok()]
            o.eT = [SB(ph, "eT%d" % i + sfx, [128, HG, 128]) for i in range(2)]; o.t_eT = [Tok(), Tok()]
            o.mT = [SB(ph, "mT%d" % i + sfx, [128, HG, 128], BF16) for i in range(2)]; o.t_mT = [Tok(), Tok()]
            o.ytmp = [SB(ph, "ytmp%d" % i + sfx, [128, 384]) for i in range(2)]; o.t_ytmp = [Tok(), Tok()]
            o.yc = [SB(ph, "yc%d" % i + sfx, [128, 384]) for i in range(2)]; o.t_yc = [Tok(), Tok()]
            o.stsb = [SB(ph, "stsb%d" % i + sfx, [128, 384]) for i in range(2)]; o.t_stsb = [Tok(), Tok()]
            o.s_f = SB(ph, "s_f" + sfx, [128, 384]); o.s_b16 = SB(ph, "s_b16" + sfx, [128, 384], BF16); o.t_s = Tok(); o.t_sb = Tok()
            dirs.append(o)
        pP = [PS(ph, "pP%d" % i, [128, 512]) for i in range(2)]; t_pP = [Tok(), Tok()]
        pX = PS(ph, "pX", [128, 4, 128], BF16); t_pX = Tok()
        pR = [PS(ph, "pR%d" % i, [128, 4, 128]) for i in range(2)]; t_pR = [Tok() for _ in range(HG)]
        pY = PS(ph, "pY", [128, 512]); t_pY = Tok()
        pO = PS(ph, "pO", [128, 512]); t_pO = Tok()
        pS = PS(ph, "pS", [128, 512]); t_pS = Tok(); t_pCB = Tok()

        def ssd_group(g, is_ctx):
            ntile = NTC if is_ctx else NT
            LL = CTX if is_ctx else L
            bw = min(512, LL)
            nblk = LL // bw
            rl = CTX if is_ctx else GRID_W
            nr = bw // rl
            src = hTc_d if is_ctx else hT_d
            dtt = dt_c if is_ctx else dt_x
            dat = da_c if is_ctx else da_x
            DMA("pool", wg[:, :, 0:384], wview(w_in_d, O_X + g * 384, O_X + (g + 1) * 384), [], [t_wg])
            DMA("pool", wg[:, :, 384:512], wview(w_in_d, O_B + g * 128, O_B + (g + 1) * 128), [], [t_wg])
            DMA("pool", wg[:, :, 512:640], wview(w_in_d, O_C + g * 128, O_C + (g + 1) * 128), [], [t_wg])
            chtile = [(O_X - O_X) // 128 + g * 3 + 0, g * 3 + 1, g * 3 + 2, 12 + g, 16 + g]
            nsub = bw // 128
            jobs = [(nb, ct) for nb in range(nblk) for ct in range(5)]

            def proj_mm(k):
                nb, ct = jobs[k]
                hb = nb % 2
                pb = k % 2
                if ct == 0:
                    DMA("sp", hTb[hb][:, :, 0:bw], src[:, :, nb * bw:(nb + 1) * bw], [], [t_hTb[hb]])
                for kc in range(KC):
                    MM(pP[pb][:, 0:bw], wg[:, kc, ct * 128:(ct + 1) * 128], hTb[hb][:, kc, 0:bw], kc == 0, kc == KC - 1,
                       [t_wg, t_hTb[hb]], [t_pP[pb]])

            def proj_post(k):
                nb, ct = jobs[k]
                pb = k % 2
                cti = chtile[ct]
                pv = pP[pb][:, 0:bw].rearrange("p (r c) -> p r c", c=rl)
                av = acc[pb][:, 0:bw].rearrange("p (r c) -> p r c", c=rl)
                TS("dve", acc[pb][:, 0:bw], pP[pb][:, 0:bw], cw[:, cti, 1:2], cb[:, cti:cti + 1], ALU.mult, ALU.add,
                   [t_pP[pb], t_cw], [t_acc[pb]])
                STT("dve", av[:, :, 1:rl], pv[:, :, 0:rl - 1], cw[:, cti, 0:1], av[:, :, 1:rl], ALU.mult, ALU.add,
                    [t_pP[pb], t_cw, t_acc[pb]], [t_acc[pb]])
                STT("dve", av[:, :, 0:rl - 1], pv[:, :, 1:rl], cw[:, cti, 2:3], av[:, :, 0:rl - 1], ALU.mult, ALU.add,
                    [t_pP[pb], t_cw, t_acc[pb]], [t_acc[pb]])
                if ct < 3:
                    ACT(xTb[pb][:, 0:bw], acc[pb][:, 0:bw], AF.Silu, [t_acc[pb]], [t_xTb[pb]])
                elif ct == 3:
                    ACT(bT[:, nb * bw:(nb + 1) * bw], acc[pb][:, 0:bw], AF.Silu, [t_acc[pb]], [t_bT[nb]])
                else:
                    ACT(cT[:, nb * bw:(nb + 1) * bw], acc[pb][:, 0:bw], AF.Silu, [t_acc[pb]], [t_cT[nb]])

            def proj_tr(k):
                nb, ct = jobs[k]
                pb = k % 2
                if ct < 3:
                    for j in range(nsub):
                        TR(pX[:, j, :], xTb[pb][:, j * 128:(j + 1) * 128], ident_b[:], [t_xTb[pb]] + C, [t_pX])
                    tl = [t_xtm[nb * nsub + j] for j in range(nsub)]
                    CP("act", x_tm[:, nb * nsub:(nb + 1) * nsub, ct * 128:(ct + 1) * 128], pX[:, 0:nsub, :], [t_pX], tl)
                elif ct == 3:
                    for j in range(nsub):
                        TR(pX[:, j, :], bT[:, nb * bw + j * 128:nb * bw + (j + 1) * 128], ident_b[:], [t_bT[nb]] + C, [t_pX])
                    tl = [t_btm[nb * nsub + j] for j in range(nsub)]
                    CP("act", b_tm[:, nb * nsub:(nb + 1) * nsub, :], pX[:, 0:nsub, :], [t_pX], tl)

            if F_PROJ:
                for k in range(len(jobs) + 1):
                    if k < len(jobs):
                        proj_mm(k)
                        proj_post(k)
                    if k >= 1:
                        proj_tr(k - 1)
            else:
                for k in range(len(jobs)):
                    proj_mm(k)
                    proj_post(k)
                    proj_tr(k)
            nn = ntile * HG
            for d in range(2):
                o = dirs[d]
                o.hsl = slice(d * NH + g * HG, d * NH + (g + 1) * HG)
                o.trib = ltri_b if d == 0 else utri_b
                o.mneg = mnegf_b if d == 0 else mnegb_b
                o.order = list(range(ntile)) if d == 0 else list(range(ntile - 1, -1, -1))
                CP("dve", o.dahi[:, 0:ntile, :], dat[:, :, o.hsl], [t_dt], [o.t_da])
                TT("dve", o.datmp[:, 0:ntile, :], dat[:, :, o.hsl], o.dahi[:, 0:ntile, :], ALU.subtract, [t_dt, o.t_da], [o.t_da])
                CP("dve", o.dalo[:, 0:ntile, :], o.datmp[:, 0:ntile, :], [o.t_da], [o.t_da])
                hi2 = o.dahi[:, 0:ntile, :].rearrange("p c h -> p (c h)")
                lo2 = o.dalo[:, 0:ntile, :].rearrange("p c h -> p (c h)")
                MM(pP[0][:, 0:nn], o.trib[:], hi2, True, False, [o.t_da] + C, [t_pP[0]])
                MM(pP[0][:, 0:nn], o.trib[:], lo2, False, True, [o.t_da] + C, [t_pP[0]])
                MM(pP[1][:, 0:nn], ones_b[:], hi2, True, False, [o.t_da] + C, [t_pP[1]])
                MM(pP[1][:, 0:nn], ones_b[:], lo2, False, True, [o.t_da] + C, [t_pP[1]])
                na2 = o.nacum[:, 0:ntile, :].rearrange("p c h -> p (c h)")
                ea2 = o.eacum[:, 0:ntile, :].rearrange("p c h -> p (c h)")
                ws2 = o.wst[:, 0:ntile, :].rearrange("p c h -> p (c h)")
                cd2 = o.cdec[:, 0:ntile, :].rearrange("p c h -> p (c h)")
                TS("dve", na2, pP[0][:, 0:nn], -1.0, None, ALU.mult, None, [t_pP[0]], [o.t_hd])
                ACT(ea2, pP[0][:, 0:nn], AF.Exp, [t_pP[0]], [o.t_hd])
                TT("dve", ws2, pP[1][:, 0:nn], na2, ALU.add, [t_pP[1], o.t_hd], [o.t_hd])
                ACT(ws2, ws2, AF.Exp, [o.t_hd], [o.t_hd])
                TT("dve", o.wst[:, 0:ntile, :], o.wst[:, 0:ntile, :], dtt[:, :, o.hsl], ALU.mult, [o.t_hd, t_dt], [o.t_hd])
                ACT(cd2, pP[1][:, 0:nn], AF.Exp, [t_pP[1]], [o.t_hd])
                if is_ctx:
                    MSET("dve", o.s_f[:], 0.0, [o.t_s])
                else:
                    CP("dve", o.s_f[:], sc_state[:, d * NG + g, :], [t_scs[d * NG + g]], [o.t_s])
                CP("act", o.s_b16[:], o.s_f[:], [o.t_s], [o.t_sb])

            rcount = [0]

            def stage_pre(d, it):
                o = dirs[d]
                c = o.order[it]
                b = it % 2
                xv = x_tm[:, c, :].rearrange("p (h q) -> p h q", q=HP)
                if not is_ctx:
                    TT("dve", o.xdt[b][:].rearrange("p (h q) -> p h q", q=HP), xv, dtt[:, c, o.hsl].unsqueeze(2).to_broadcast([128, HG, HP]),
                       ALU.mult, [t_xtm[c], t_dt], [o.t_xdt[b]])
                TT("dve", o.xw[b][:].rearrange("p (h q) -> p h q", q=HP), xv, o.wst[:, c, :].unsqueeze(2).to_broadcast([128, HG, HP]),
                   ALU.mult, [t_xtm[c], o.t_hd], [o.t_xw[b]])

            def stage_a(d, it):
                o = dirs[d]
                c = o.order[it]
                b = it % 2
                nb = c // nsub
                xv = x_tm[:, c, :].rearrange("p (h q) -> p h q", q=HP)
                if not is_ctx:
                    for h in range(HG):
                        rb = rcount[0] % 4
                        rcount[0] += 1
                        if rb < 2:
                            prv = pR[rb][:, 0, :]; tpr = t_pR[rb]
                        else:
                            prv = pP[rb - 2][:, 0:128]; tpr = t_pP[rb - 2]
                        MM(prv, o.dahi[:, c, h:h + 1].to_broadcast([128, 128]), o.trib[:], True, False, [o.t_da] + C, [tpr])
                        MM(prv, o.dalo[:, c, h:h + 1].to_broadcast([128, 128]), o.trib[:], False, False, [o.t_da] + C, [tpr])
                        MM(prv, ident_b[:], o.mneg[:], False, True, C, [tpr])
                        ACT(o.eT[b][:, h, :], prv, AF.Exp, [tpr, o.t_hd], [o.t_eT[b]], bias=o.nacum[:, c, h:h + 1])
                    csl = slice(c * 128, (c + 1) * 128)
                    MM(pS[:, 384:512], bT[:, csl], cT[:, csl], True, True, [t_bT[nb], t_cT[nb]], [t_pCB])
                    CP("act", o.cbT[b][:], pS[:, 384:512], [t_pCB], [o.t_cbT[b]])
                MM(pS[:, 0:384], b_tm[:, c, :], o.xw[b][:], True, True, [t_btm[c], o.t_xw[b]], [t_pS])
                CP("act", o.stsb[b][:], pS[:, 0:384], [t_pS], [o.t_stsb[b]])
                if not is_ctx:
                    TT("dve", o.mT[b][:], o.eT[b][:], o.cbT[b][:].unsqueeze(1).to_broadcast([128, HG, 128]), ALU.mult,
                       [o.t_eT[b], o.t_cbT[b]], [o.t_mT[b]])

            def stage_b(d, it):
                o = dirs[d]
                c = o.order[it]
                b = it % 2
                nb = c // nsub
                if not is_ctx:
                    csl = slice(c * 128, (c + 1) * 128)
                    for h in range(HG):
                        hh = g * HG + h
                        last = (d != 0) or not F_IDD
                        MM(pY[:, h * HP:(h + 1) * HP], o.mT[b][:, h, :], o.xdt[b][:, h * HP:(h + 1) * HP], True, last,
                           [o.t_mT[b], o.t_xdt[b]], [t_pY])
                        if d == 0 and F_IDD:
                            MM(pY[:, h * HP:(h + 1) * HP], identD[:, hh, :], x_tm[:, c, h * HP:(h + 1) * HP], False, True,
                               [t_idD, t_xtm[c]], [t_pY])
                    MM(pO[:, 0:384], cT[:, csl], o.s_b16[:], True, True, [t_cT[nb], o.t_sb], [t_pO])
                    TT("dve", o.ytmp[b][:].rearrange("p (h q) -> p h q", q=HP), pO[:, 0:384].rearrange("p (h q) -> p h q", q=HP),
                       o.eacum[:, c, :].unsqueeze(2).to_broadcast([128, HG, HP]), ALU.mult, [t_pO, o.t_hd], [o.t_ytmp[b]])
                    TT("dve", o.yc[b][:], o.ytmp[b][:], pY[:, 0:384], ALU.add, [o.t_ytmp[b], t_pY], [o.t_yc[b]])
                    if d == 0 and not F_IDD:
                        xv = x_tm[:, c, :].rearrange("p (h q) -> p h q", q=HP)
                        TT("dve", o.ytmp[b][:].rearrange("p (h q) -> p h q", q=HP), xv,
                           dskip_bc[:, g * HG:(g + 1) * HG].unsqueeze(2).to_broadcast([128, HG, HP]), ALU.mult,
                           [t_xtm[c], t_dsk, o.t_yc[b]], [o.t_ytmp[b]])
                        TT("dve", o.yc[b][:], o.yc[b][:], o.ytmp[b][:], ALU.add, [o.t_ytmp[b]], [o.t_yc[b]])
                    ydst = y_d[c * 128:(c + 1) * 128, g * 384:(g + 1) * 384]
                    if not y_written[g][c]:
                        y_written[g][c] = True
                        DMA("sp", ydst, o.yc[b][:], [o.t_yc[b]], [t_yd[g][c]])
                    else:
                        S.dma("pool", lambda e, ydst=ydst, src_=o.yc[b][:]: e.dma_start(out=ydst, in_=src_, accum_op=ALU.add),
                              reads=[o.t_yc[b]], writes=[t_yd[g][c]])
                TT("dve", o.s_f[:].rearrange("p (h q) -> p h q", q=HP), o.s_f[:].rearrange("p (h q) -> p h q", q=HP),
                   o.cdec[:, c, :].unsqueeze(2).to_broadcast([128, HG, HP]), ALU.mult, [o.t_hd, o.t_s], [o.t_s])
                TT("dve", o.s_f[:], o.s_f[:], o.stsb[b][:], ALU.add, [o.t_stsb[b], o.t_s], [o.t_s])
                CP("act", o.s_b16[:], o.s_f[:], [o.t_s], [o.t_sb])

            for d in range(2):
                stage_pre(d, 0)
            for it in range(ntile + 1):
                for d in range(2):
                    if it < ntile:
                        stage_a(d, it)
                    if it >= 1:
                        stage_b(d, it - 1)
                    if it + 1 < ntile:
                        stage_pre(d, it + 1)
            if is_ctx:
                for d in range(2):
                    CP("dve", sc_state[:, d * NG + g, :], dirs[d].s_f[:], [dirs[d].t_s], [t_scs[d * NG + g]])

        y_written = [[False] * NT for _ in range(NG)]
        t_yd = [[Tok() for _ in range(NT)] for _ in range(NG)]
        for g in range(NG):
            ssd_group(g, True)
        for g in range(NG):
            ssd_group(g, False)
        S.barrier()
    if debug:
        sc_dbg = nc.dram_tensor("sc_dbg", [128, 2 * NG, 384], F32, kind="ExternalOutput").ap()
        DMA("sp", sc_dbg, sc_state[:], t_scs, [])
        S.barrier()
    if stop_after == "G":
        return finish(nc, S, top, out_d, dbg=None)
    stackG.close()

    fT = SB(top, "fT", [128, 4, L], BF16); t_fT = Tok()
    with contextlib.ExitStack() as ph:
        ck = SB(ph, "ckb", [128, 128], BF16); sk = SB(ph, "skb", [128, 128], BF16); t_ck = Tok()
        sgn = SB(ph, "sgnb", [1, H2], BF16)
        ue = SB(ph, "ue", [128, 4, H2], BF16); uo = SB(ph, "uo", [128, 4, H2], BF16); t_ue = Tok()
        u2048 = SB(ph, "u2048", [1, 512], BF16); t_u2 = Tok()
        vv = SB(ph, "vv", [128, 4, 4]); vb16 = SB(ph, "vb16", [128, 4], BF16); t_vv = Tok()
        pU = [PS(ph, "pU%d" % i, [128, 512]) for i in range(2)]; t_pU = [Tok(), Tok()]
        pFc = [PS(ph, "pFc%d" % i, [128, 512]) for i in range(2)]; t_pFc = [Tok(), Tok()]
        pFs = [PS(ph, "pFs%d" % i, [128, 512]) for i in range(2)]; t_pFs = [Tok(), Tok()]
        ph2 = contextlib.ExitStack()
        wu = SB(ph2, "wu", [128, KC, 512], BF16); t_wu = Tok()
        hTb = [SB(ph2, "uhTb%d" % i, [128, KC, 512], BF16) for i in range(2)]; t_hTb = [Tok(), Tok()]
        uT = SB(ph2, "uT", [128, 4, L], BF16); t_uT = Tok()
        DMA("pool", wu[:], wview(w_in_d, 0, 512), [], [t_wu])
        DMA("pool", ck[:], ck_d, [], [t_ck])
        DMA("pool", sk[:], sk_d, [], [t_ck])
        DMA("pool", sgn[:], sgn_d, [], [t_ck])
        for nb in range(NB):
            hb = nb % 2
            DMA("sp", hTb[hb][:], hT_d[:, :, nb * 512:(nb + 1) * 512], [], [t_hTb[hb]])
            for g in range(4):
                pb = g % 2
                for kc in range(KC):
                    MM(pU[pb][:], wu[:, kc, g * 128:(g + 1) * 128], hTb[hb][:, kc, :], kc == 0, kc == KC - 1, [t_wu, t_hTb[hb]], [t_pU[pb]])
                CP("act", uT[:, g, nb * 512:(nb + 1) * 512], pU[pb][:], [t_pU[pb]], [t_uT])
        for g in range(4):
            rev = uT[:, g, H2 + 1:L][:, ::-1]
            TT("dve", ue[:, g, 1:H2], uT[:, g, 1:H2], rev, ALU.add, [t_uT], [t_ue])
            TT("dve", uo[:, g, 1:H2], uT[:, g, 1:H2], rev, ALU.subtract, [t_uT], [t_ue])
            CP("dve", ue[:, g, 0:1], uT[:, g, 0:1], [t_uT], [t_ue])
            MSET("dve", uo[:, g, 0:1], 0.0, [t_ue])
            uv = uT[:, g, :].rearrange("p (n two) -> p two n", two=2)
            RED("dve", vv[:, g, 0:2], uv, ALU.add, [t_uT], [t_vv])
            TT("dve", vv[:, g, 2:3], vv[:, g, 0:1], vv[:, g, 1:2], ALU.subtract, [t_vv], [t_vv])
            CP("dve", vb16[:, g:g + 1], vv[:, g, 2:3], [t_vv], [t_vv])
        for g in range(4):
            MM(pU[0][0:1, g * 128:(g + 1) * 128], uT[:, g, H2:H2 + 1], ck[:], True, True, [t_uT, t_ck], [t_pU[0]])
        CP("act", u2048[:], pU[0][0:1, :], [t_pU[0]], [t_u2])
        for g in range(4):
            MM(pU[1][:, g:g + 1], ck[:], vb16[:, g:g + 1], True, True, [t_vv, t_ck], [t_pU[1]])
        for g in range(4):
            CP("act", fT[:, g, H2:H2 + 1], pU[1][:, g:g + 1], [t_pU[1]], [t_fT])
        S.barrier()
        ph2.close()
        e_tm = SB(ph, "e_tm", [128, NHC, 512], BF16); o_tm = SB(ph, "o_tm", [128, NHC, 512], BF16); t_eo = Tok()
        cblk = [SB(ph, "cblk%d" % i, [128, NHC, PW], BF16) for i in range(1)] * 2; t_cblk = [Tok()] * 2
        sblk = [SB(ph, "sblk%d" % i, [128, NHC, PW], BF16) for i in range(1)] * 2; t_sblk = [Tok()] * 2
        sfc = [SB(ph, "sfc%d" % i, [128, PW]) for i in range(2)]; sfs = [SB(ph, "sfs%d" % i, [128, PW]) for i in range(2)]
        t_sf = [Tok(), Tok()]
        for n in range(NHC):
            for (srcT, mat, dst, pb) in ((ue, ck, e_tm, 0), (uo, sk, o_tm, 1)):
                for g in range(4):
                    MM(pU[pb][:, g * 128:(g + 1) * 128], srcT[:, g, n * 128:(n + 1) * 128], mat[:], True, True, [t_ue, t_ck], [t_pU[pb]])
                CP("act", dst[:, n, :], pU[pb][:], [t_pU[pb]], [t_eo])
        for pc in range(NPC):
            cbf = pc % 2
            DMA("pool", cblk[cbf][:], cmat_d.rearrange("(n p) q -> p n q", p=128)[:, :, pc * PW:(pc + 1) * PW], [], [t_cblk[cbf]])
            DMA("pool", sblk[cbf][:], smat_d.rearrange("(n p) q -> p n q", p=128)[:, :, pc * PW:(pc + 1) * PW], [], [t_sblk[cbf]])
            for g in range(4):
                pb = g % 2
                for n in range(NHC):
                    MM(pFc[pb][:, 0:PW], e_tm[:, n, g * 128:(g + 1) * 128], cblk[cbf][:, n, :], n == 0, False, [t_eo, t_cblk[cbf]], [t_pFc[pb]])
                MM(pFc[pb][:, 0:PW], u2048[0:1, g * 128:(g + 1) * 128], sgn[0:1, pc * PW:(pc + 1) * PW], False, True, [t_u2, t_ck], [t_pFc[pb]])
                for n in range(NHC):
                    MM(pFs[pb][:, 0:PW], o_tm[:, n, g * 128:(g + 1) * 128], sblk[cbf][:, n, :], n == 0, n == NHC - 1, [t_eo, t_sblk[cbf]], [t_pFs[pb]])
                CP("act", sfc[pb][:], pFc[pb][:, 0:PW], [t_pFc[pb]], [t_sf[pb]])
                CP("act", sfs[pb][:], pFs[pb][:, 0:PW], [t_pFs[pb]], [t_sf[pb]])
                p0 = pc * PW
                TT("dve", fT[:, g, p0:p0 + PW], sfc[pb][:], sfs[pb][:], ALU.subtract, [t_sf[pb]], [t_fT])
                lo = 1 if pc == 0 else 0
                TT("dve", fT[:, g, L - p0 - PW + 1:L - p0 - lo + 1], sfc[pb][:, lo:PW][:, ::-1], sfs[pb][:, lo:PW][:, ::-1], ALU.add,
                   [t_sf[pb]], [t_fT])
        S.barrier()
    if debug:
        fT_dbg = nc.dram_tensor("fT_dbg", [128, 4, L], BF16, kind="ExternalOutput").ap()
        DMA("sp", fT_dbg, fT[:], [t_fT], [])
    if stop_after == "U":
        return finish(nc, S, top, out_d, dbg=None)

    with contextlib.ExitStack() as ph:
        wz = SB(ph, "wz", [128, KC, DI], BF16); t_wz = Tok()
        wss = SB(ph, "wss", [128, 12, D], BF16); t_wss = Tok()
        ssdn = SB(ph, "ssdn", [128, DI]); t_ssdn = Tok()
        hTb = [SB(ph, "zhTb%d" % i, [128, KC, 512], BF16) for i in range(2)]; t_hTb = [Tok(), Tok()]
        zs = [SB(ph, "zs%d" % i, [128, DI]) for i in range(2)]; t_zs = [Tok(), Tok()]
        yt = [SB(ph, "yt%d" % i, [128, DI]) for i in range(2)]; t_yt = [Tok(), Tok()]
        junk = SB(ph, "zjunk", [128, DI], BF16); t_junk = Tok()
        st4 = [SB(ph, "zst4%d" % i, [128, 4]) for i in range(2)]; t_st = [Tok(), Tok()]
        yzn = [SB(ph, "yzn%d" % i, [128, DI], BF16) for i in range(2)]; t_yzn = [Tok(), Tok()]
        yzT = [SB(ph, "yzT0", [128, 12, 512], BF16)] * 2; t_yzT = [Tok()] * 2
        sbrT = [SB(ph, "sbrT0", [128, KC, 512], BF16)] * 2; t_sbrT = [Tok()] * 2
        pZ = [PS(ph, "pZ%d" % i, [128, 512]) for i in range(3)]; t_pZ = [Tok(), Tok(), Tok()]
        pTz = PS(ph, "pTz", [128, 2048], BF16); t_pTz = Tok()
        pSb = [PS(ph, "pSb%d" % i, [128, 512]) for i in range(2)]; t_pSb = [Tok(), Tok()]
        DMA("pool", wz[:], wview(w_in_d, O_Z, O_Z + DI), [], [t_wz])
        DMA("pool", wss[:], wview(w_ssd_d, 0, D), [], [t_wss])
        DMA("sp", ssdn[:], ssdn_d.to_broadcast([128, DI]), [], [t_ssdn])
        def z1_stage1(i):
            nb, j = divmod(i, 4)
            hb = nb % 2
            b = i % 2
            if j == 0:
                DMA("sp", hTb[hb][:], hT_d[:, :, nb * 512:(nb + 1) * 512], [], [t_hTb[hb]])
            DMA("act", yt[b][:], y_d[i * 128:(i + 1) * 128, :], [], [t_yt[b]])
            for zc in range(3):
                for kc in range(KC):
                    MM(pZ[zc][:], hTb[hb][:, kc, j * 128:(j + 1) * 128], wz[:, kc, zc * 512:(zc + 1) * 512], kc == 0, kc == KC - 1,
                       [t_hTb[hb], t_wz], [t_pZ[zc]])
                ACT(zs[b][:, zc * 512:(zc + 1) * 512], pZ[zc][:], AF.Silu, [t_pZ[zc]], [t_zs[b]])
            TT("dve", zs[b][:], zs[b][:], yt[b][:], ALU.mult, [t_zs[b], t_yt[b]], [t_zs[b]])
            ACT(junk[:], zs[b][:], AF.Square, [t_zs[b]], [t_junk, t_st[b]], accum_out=st4[b][:, 0:1])
            TS("dve", st4[b][:, 1:2], st4[b][:, 0:1], 1.0 / DI, EPS, ALU.mult, ALU.add, [t_st[b]], [t_st[b]])
            ACT(st4[b][:, 2:3], st4[b][:, 1:2], AF.Sqrt, [t_st[b]], [t_st[b]])
            RECIP(st4[b][:, 3:4], st4[b][:, 2:3], [t_st[b]], [t_st[b]])
            STT("dve", yzn[b][:], zs[b][:], st4[b][:, 3:4], ssdn[:], ALU.mult, ALU.mult, [t_zs[b], t_st[b], t_ssdn], [t_yzn[b]])

        def z1_stage2(i):
            nb, j = divmod(i, 4)
            hb = nb % 2
            b = i % 2
            for ctile in range(12):
                TR(pTz[:, ctile * 128:(ctile + 1) * 128], yzn[b][:, ctile * 128:(ctile + 1) * 128], ident_b[:], [t_yzn[b]] + C, [t_pTz])
            CP("act", yzT[hb][:, :, j * 128:(j + 1) * 128], pTz[:, 0:1536].rearrange("p (c t) -> p c t", c=12), [t_pTz], [t_yzT[hb]])
            if j == 3:
                for dc in range(KC):
                    pb = dc % 2
                    for ctile in range(12):
                        MM(pSb[pb][:], wss[:, ctile, dc * 128:(dc + 1) * 128], yzT[hb][:, ctile, :], ctile == 0, ctile == 11,
                           [t_wss, t_yzT[hb]], [t_pSb[pb]])
                    CP("act", sbrT[hb][:, dc, :], pSb[pb][:], [t_pSb[pb]], [t_sbrT[hb]])
                DMA("sp", sbr_d[:, :, nb * 512:(nb + 1) * 512], sbrT[hb][:], [t_sbrT[hb]], [])

        for i in range(NT + 1):
            if i < NT:
                z1_stage1(i)
            if i >= 1:
                z1_stage2(i - 1)
        S.barrier()
    if stop_after == "Z1":
        return finish(nc, S, top, out_d, dbg=None)

    logits = SB(top, "logits", [128, NT, NE]); t_lg = Tok()
    with contextlib.ExitStack() as ph:
        wgt = SB(ph, "wgt", [128, KC, 2 * D], BF16); t_wgt = Tok()
        wfo = SB(ph, "wfo", [128, 4, D], BF16); t_wfo = Tok()
        wo = SB(ph, "wo", [128, KC, D], BF16); t_wo = Tok()
        wr = SB(ph, "wr", [128, KC, NE]); t_wr = Tok()
        bg = SB(ph, "bg", [128, 16]); t_bg = Tok()
        hTb = [SB(ph, "yhTb%d" % i, [128, KC, 512], BF16) for i in range(2)]; t_hTb = [Tok(), Tok()]
        sbrT = [SB(ph, "ysbrT0", [128, KC, 512], BF16)] * 2; t_sbrT = [Tok()] * 2
        gT = SB(ph, "gT", [128, 16, 512], BF16); t_gT = Tok()
        m1 = [SB(ph, "m1%d" % i, [128, 512]) for i in range(2)]; t_m1 = [Tok(), Tok()]
        m2 = [SB(ph, "m20", [128, 512])] * 2; t_m2 = [Tok()] * 2
        mrg = [SB(ph, "mrg0", [128, KC, 512], BF16)] * 2; t_mrg = [Tok()] * 2
        mix = [SB(ph, "mix%d" % i, [128, D]) for i in range(2)]; t_mix = [Tok(), Tok()]
        xt = [SB(ph, "zxt%d" % i, [128, D]) for i in range(2)]; t_xt = [Tok(), Tok()]
        hx = [SB(ph, "hx%d" % i, [128, D]) for i in range(2)]; t_hx = [Tok(), Tok()]
        h2f = [SB(ph, "h2f%d" % i, [128, D]) for i in range(2)]; t_h2f = [Tok(), Tok()]
        h2b = [SB(ph, "h2b0", [128, 512])] * 2; t_h2b = [Tok()] * 2
        h2T = SB(ph, "h2T", [128, KC, 128]); t_h2T = Tok()
        junk = SB(ph, "yjunk", [128, D], BF16); t_junk = Tok()
        st8 = [SB(ph, "st8%d" % i, [128, 8]) for i in range(2)]; t_st = [Tok(), Tok()]
        pA = [PS(ph, "pA%d" % i, [128, 512]) for i in range(4)]; t_pA = [Tok() for _ in range(4)]
        pH = PS(ph, "pH", [128, KC, 128]); t_pH = Tok()
        pL = PS(ph, "pL", [128, 512]); t_pL = Tok()
        DMA("pool", wgt[:], wview(w_in_d, O_GATE, O_GATE + 2 * D), [], [t_wgt])
        DMA("pool", wfo[:], wview(w_four_d, 0, D), [], [t_wfo])
        DMA("pool", wo[:], wview(w_out_d, 0, D), [], [t_wo])
        DMA("sp", wr[:], wview(w_r_d, 0, NE), [], [t_wr])
        DMA("sp", bg[:], bgate_d, [], [t_bg])
        for nb in range(NB):
            hb = nb % 2
            DMA("sp", hTb[hb][:], hT_d[:, :, nb * 512:(nb + 1) * 512], [], [t_hTb[hb]])
            DMA("act", sbrT[hb][:], sbr_d[:, :, nb * 512:(nb + 1) * 512], [], [t_sbrT[hb]])
            for gi in range(16):
                pb = gi % 2
                for kc in range(KC):
                    MM(pA[pb][:], wgt[:, kc, gi * 128:(gi + 1) * 128], hTb[hb][:, kc, :], kc == 0, kc == KC - 1, [t_wgt, t_hTb[hb]], [t_pA[pb]])
                ACT(gT[:, gi, :], pA[pb][:], AF.Sigmoid, [t_pA[pb], t_bg], [t_gT], bias=bg[:, gi:gi + 1])
            for dc in range(KC):
                pb = 2 + dc % 2
                b = dc % 2
                for g in range(4):
                    MM(pA[pb][:], wfo[:, g, dc * 128:(dc + 1) * 128], fT[:, g, nb * 512:(nb + 1) * 512], g == 0, g == 3, [t_wfo, t_fT], [t_pA[pb]])
                TT("dve", m1[b][:], pA[pb][:], gT[:, dc, :], ALU.mult, [t_pA[pb], t_gT], [t_m1[b]])
                TT("dve", m2[b][:], sbrT[hb][:, dc, :], gT[:, 8 + dc, :], ALU.mult, [t_sbrT[hb], t_gT], [t_m2[b]])
                TT("dve", mrg[hb][:, dc, :], m1[b][:], m2[b][:], ALU.add, [t_m1[b], t_m2[b]], [t_mrg[hb]])
            def z2_mix(j, nb=nb, hb=hb):
                i = nb * 4 + j
                b = i % 2
                DMA("sp", xt[b][:], x_d[i * 128:(i + 1) * 128, :], [], [t_xt[b]])
                for dh in range(2):
                    pb = dh
                    for kc in range(KC):
                        MM(pA[pb][:], mrg[hb][:, kc, j * 128:(j + 1) * 128], wo[:, kc, dh * 512:(dh + 1) * 512], kc == 0, kc == KC - 1,
                           [t_mrg[hb], t_wo], [t_pA[pb]])
                    CP("act", mix[b][:, dh * 512:(dh + 1) * 512], pA[pb][:], [t_pA[pb]], [t_mix[b]])
                ACT(junk[:], mix[b][:], AF.Square, [t_mix[b]], [t_junk, t_st[b]], accum_out=st8[b][:, 0:1])
                TS("dve", st8[b][:, 1:2], st8[b][:, 0:1], 1.0 / D, EPS, ALU.mult, ALU.add, [t_st[b]], [t_st[b]])
                ACT(st8[b][:, 2:3], st8[b][:, 1:2], AF.Sqrt, [t_st[b]], [t_st[b]])
                RECIP(st8[b][:, 3:4], st8[b][:, 2:3], [t_st[b]], [t_st[b]])
                STT("dve", mix[b][:], mix[b][:], st8[b][:, 3:4], g1row[:], ALU.mult, ALU.mult, [t_mix[b], t_st[b], t_mod], [t_mix[b]])
                TT("dve", hx[b][:], mix[b][:], xt[b][:], ALU.add, [t_mix[b], t_xt[b]], [t_hx[b]])
                DMA("act", hx_d[i * 128:(i + 1) * 128, :], hx[b][:], [t_hx[b]], [])
                ACT(junk[:], hx[b][:], AF.Square, [t_hx[b]], [t_junk, t_st[b]], accum_out=st8[b][:, 4:5])
                TS("dve", st8[b][:, 5:6], st8[b][:, 4:5], 1.0 / D, EPS, ALU.mult, ALU.add, [t_st[b]], [t_st[b]])
                ACT(st8[b][:, 6:7], st8[b][:, 5:6], AF.Sqrt, [t_st[b]], [t_st[b]])
                RECIP(st8[b][:, 7:8], st8[b][:, 6:7], [t_st[b]], [t_st[b]])
                STT("dve", h2f[b][:], hx[b][:], st8[b][:, 7:8], a2row[:], ALU.mult, ALU.mult, [t_hx[b], t_st[b], t_mod], [t_h2f[b]])
                TT("dve", h2f[b][:], h2f[b][:], b2row[:], ALU.add, [t_mod], [t_h2f[b]])
                CP("act", h2b[b][:].bitcast(BF16), h2f[b][:], [t_h2f[b]], [t_h2b[b]])
                DMA("act", h2_d[i * 128:(i + 1) * 128, :], h2b[b][:], [t_h2b[b]], [])

            def z2_router(j, nb=nb):
                i = nb * 4 + j
                b = i % 2
                for kc in range(KC):
                    TR(pH[:, kc, :], h2f[b][:, kc * 128:(kc + 1) * 128], ident_f[:], [t_h2f[b]] + C, [t_pH])
                CP("act", h2T[:], pH[:], [t_pH], [t_h2T])
                for kc in range(KC):
                    MM(pL[:, 0:NE], h2T[:, kc, :], wr[:, kc, :], kc == 0, kc == KC - 1, [t_h2T, t_wr], [t_pL])
                CP("dve", logits[:, i, :], pL[:, 0:NE], [t_pL], [t_lg])

            for j in range(5):
                if j < 4:
                    z2_mix(j)
                if j >= 1:
                    z2_router(j - 1)
        S.barrier()
    if stop_after == "Z2":
        return finish(nc, S, top, out_d, dbg=None)

    aff = SB(top, "aff", [128, NT, NE]); t_aff = Tok()
    t_xg = Tok(); t_ffn = Tok()
    offi = SB(top, "offi", [128, NT * NE], I32); t_offi = Tok()
    tokid = SB(top, "tokid", [128, NT], I32); t_tokid = Tok()
    S.op("pool", lambda e, o_=tokid[:]: e.iota(o_, pattern=[[128, NT]], base=0, channel_multiplier=1), writes=[t_tokid])
    with contextlib.ExitStack() as ph:
        mx = SB(ph, "mx", [128, NT]); t_mx = Tok()
        msk = SB(ph, "msk", [128, NT, NE]); t_msk = Tok()
        part = SB(ph, "part", [128, NE]); t_part = Tok()
        lo_t = SB(ph, "lo_t", [128, NE]); mid = SB(ph, "mid", [128, NE]); sel = SB(ph, "sel", [128, NE]); t_lo = Tok()
        posf = SB(ph, "posf", [128, NT, NE]); t_pos = Tok()
        base = SB(ph, "base", [128, NT, NE]); t_base = Tok()
        eoff = SB(ph, "eoff", [128, NE]); t_eoff = Tok()
        pC_ = PS(ph, "pC_", [128, 512]); t_pC = Tok()
        pW = PS(ph, "pW", [128, 512]); t_pW = Tok()
        pTt = PS(ph, "pTt", [128, 512]); t_pTt = Tok()
        zt = SB(ph, "zt", [128, D]); t_zt = Tok()
        MSET("pool", zt[:], 0.0, [t_zt])
        for i in range(NT):
            DMA("act", ffn_d[i * 128:(i + 1) * 128, :], zt[:], [t_zt], [t_ffn])
        RED("dve", mx[:], logits[:], ALU.max, [t_lg], [t_mx])
        TT("dve", aff[:], logits[:], mx[:].unsqueeze(2).to_broadcast([128, NT, NE]), ALU.subtract, [t_lg, t_mx], [t_aff])
        ACT(aff[:], aff[:], AF.Exp, [t_aff], [t_aff])
        RED("dve", mx[:], aff[:], ALU.add, [t_aff], [t_mx])
        RECIP(mx[:], mx[:], [t_mx], [t_mx])
        TT("dve", aff[:], aff[:], mx[:].unsqueeze(2).to_broadcast([128, NT, NE]), ALU.mult, [t_mx, t_aff], [t_aff])
        MSET("dve", lo_t[:], 0.0, [t_lo])
        w = 0.5
        for it in range(30):
            TS("dve", mid[:], lo_t[:], w, None, ALU.add, None, [t_lo], [t_lo])
            TT("dve", msk[:], aff[:], mid[:].unsqueeze(1).to_broadcast([128, NT, NE]), ALU.is_ge, [t_aff, t_lo], [t_msk])
            RED("dve", part[:], msk[:].rearrange("p i e -> p e i"), ALU.add, [t_msk], [t_part])
            MM(pC_[:, 0:NE], ones_f[:], part[:], True, True, [t_part] + C, [t_pC])
            TSS(sel[:], pC_[:, 0:NE], CAP - 0.5, ALU.is_ge, [t_pC], [t_lo])
            STT("dve", lo_t[:], sel[:], w, lo_t[:], ALU.mult, ALU.add, [t_lo], [t_lo])
            w *= 0.5
        TT("dve", msk[:], aff[:], lo_t[:].unsqueeze(1).to_broadcast([128, NT, NE]), ALU.is_ge, [t_aff, t_lo], [t_msk])
        m2d = msk[:].rearrange("p i e -> p (i e)")
        nn = NT * NE
        MM(pW[:, 0:nn], slt_f[:], m2d, True, True, [t_msk] + C, [t_pW])
        MM(pTt[:, 0:nn], ones_f[:], m2d, True, True, [t_msk] + C, [t_pTt])
        MSET("dve", base[:, 0, :], 0.0, [t_base])
        for i in range(1, NT):
            TT("dve", base[:, i, :], base[:, i - 1, :], pTt[:, (i - 1) * NE:i * NE], ALU.add, [t_pTt, t_base], [t_base])
        TT("dve", posf[:].rearrange("p i e -> p (i e)"), base[:].rearrange("p i e -> p (i e)"), pW[:, 0:nn], ALU.add, [t_base, t_pW], [t_pos])
        TSS(base[:], posf[:], CAP - 0.5, ALU.is_lt, [t_pos], [t_base])
        TT("dve", msk[:], msk[:], base[:], ALU.mult, [t_base, t_msk], [t_msk])
        S.op("pool", lambda e, o_=eoff[:].bitcast(I32): e.iota(o_, pattern=[[CAP, NE]], base=0, channel_multiplier=0), writes=[t_eoff])
        CP("dve", base[:, 0, :], eoff[:].bitcast(I32), [t_eoff, t_base], [t_base])
        TS("dve", base[:, 0, :], base[:, 0, :], -BIG, None, ALU.add, None, [t_base], [t_base])
        TT("dve", posf[:], posf[:], base[:, 0, :].unsqueeze(1).to_broadcast([128, NT, NE]), ALU.add, [t_base, t_pos], [t_pos])
        TT("dve", posf[:], posf[:], msk[:], ALU.mult, [t_msk, t_pos], [t_pos])
        TS("dve", posf[:], posf[:], BIG, None, ALU.add, None, [t_pos], [t_pos])
        CP("dve", offi[:], posf[:].rearrange("p i e -> p (i e)"), [t_pos], [t_offi])
        S.barrier()

    with contextlib.ExitStack() as ph:
        phs = contextlib.ExitStack()
        rowt = [SB(phs, "rowt%d" % i, [128, RW]) for i in range(2)]; t_rowt = [Tok(), Tok()]
        for b in range(2):
            MSET("pool", rowt[b][:], 0.0, [t_rowt[b]])
        for i in range(NT):
            b = i % 2
            DMA("sp", rowt[b][:, 0:512], h2_d[i * 128:(i + 1) * 128, :], [], [t_rowt[b]])
            CP("dve", rowt[b][:, 512:528], aff[:, i, :], [t_aff], [t_rowt[b]])
            CP("dve", rowt[b][:, 528:529].bitcast(I32), tokid[:, i:i + 1], [t_tokid], [t_rowt[b]])
            for ex in range(NE):
                col = i * NE + ex
                S.dma("pool", lambda e, src_=rowt[b][:, :], off_=offi[:, col:col + 1]: e.indirect_dma_start(
                    out=xg_d, out_offset=bass.IndirectOffsetOnAxis(ap=off_, axis=0),
                    in_=src_, in_offset=None, bounds_check=S.reg(e, NE * CAP - 1), oob_is_err=False),
                    reads=[t_rowt[b], t_offi], writes=[])
        S.barrier()
        phs.close()
        wgb = [SB(ph, "wgb%d" % i, [128, KC, D], BF16) for i in range(2)]; t_wgb = [Tok(), Tok()]
        wub = [SB(ph, "wub%d" % i, [128, KC, D], BF16) for i in range(2)]; t_wub = [Tok(), Tok()]
        wdb = [SB(ph, "wdb%d" % i, [128, KC, D], BF16) for i in range(2)]; t_wdb = [Tok(), Tok()]
        xg = [SB(ph, "xg%d" % i, [128, JT, RW]) for i in range(2)]; t_xgs = [Tok(), Tok()]
        xgT = [SB(ph, "xgT%d" % i, [128, KC, CAP], BF16) for i in range(2)]; t_xgT = [Tok(), Tok()]
        sg = [SB(ph, "sg%d" % i, [128, CAP]) for i in range(2)]; t_sg = [Tok(), Tok()]
        hid = SB(ph, "hid", [128, KC, CAP], BF16); t_hid = Tok()
        ysb = [SB(ph, "ysb%d" % i, [128, D]) for i in range(2)]; t_ysb = [Tok(), Tok()]
        pXt = PS(ph, "pXt", [128, D], BF16); t_pXt = Tok()
        pG = [PS(ph, "pG%d" % i, [128, 512]) for i in range(2)]; t_pG = [Tok(), Tok()]
        pUp = [PS(ph, "pUp%d" % i, [128, 512]) for i in range(2)]; t_pUp = [Tok(), Tok()]
        pD = [PS(ph, "pD%d" % i, [128, 512]) for i in range(2)]; t_pD = [Tok(), Tok()]
        ncol = (CAP + 511) // 512

        def ex_T(ex):
            eb = ex % 2
            DMA("pool", wgb[eb][:], weg_d[ex].rearrange("(kc p) n -> p kc n", p=128), [], [t_wgb[eb]])
            DMA("pool", wub[eb][:], weu_d[ex].rearrange("(kc p) n -> p kc n", p=128), [], [t_wub[eb]])
            DMA("pool", wdb[eb][:], wed_d[ex].rearrange("(kc p) n -> p kc n", p=128), [], [t_wdb[eb]])
            DMA("sp", xg[eb][:], xg_d[ex * CAP:(ex + 1) * CAP, :].rearrange("(j p) w -> p j w", p=128), [], [t_xgs[eb]])
            for jt in range(JT):
                xrow = xg[eb][:, jt, 0:512].bitcast(BF16)
                for kc in range(KC):
                    TR(pXt[:, kc * 128:(kc + 1) * 128], xrow[:, kc * 128:(kc + 1) * 128], ident_b[:], [t_xgs[eb]] + C, [t_pXt])
                CP("act", xgT[eb][:, :, jt * 128:(jt + 1) * 128], pXt[:].rearrange("p (k t) -> p k t", k=KC), [t_pXt], [t_xgT[eb]])

        def ex_GU(ex):
            eb = ex % 2
            for fc in range(KC):
                for cc in range(ncol):
                    c0 = cc * 512
                    c1 = min(CAP, c0 + 512)
                    pb = (fc * ncol + cc) % 2
                    for kc in range(KC):
                        MM(pG[pb][:, 0:c1 - c0], wgb[eb][:, kc, fc * 128:(fc + 1) * 128], xgT[eb][:, kc, c0:c1], kc == 0, kc == KC - 1,
                           [t_wgb[eb], t_xgT[eb]], [t_pG[pb]])
                    for kc in range(KC):
                        MM(pUp[pb][:, 0:c1 - c0], wub[eb][:, kc, fc * 128:(fc + 1) * 128], xgT[eb][:, kc, c0:c1], kc == 0, kc == KC - 1,
                           [t_wub[eb], t_xgT[eb]], [t_pUp[pb]])
                    ACT(sg[pb][:, 0:c1 - c0], pG[pb][:, 0:c1 - c0], AF.Silu, [t_pG[pb]], [t_sg[pb]])
                    TT("dve", hid[:, fc, c0:c1], sg[pb][:, 0:c1 - c0], pUp[pb][:, 0:c1 - c0], ALU.mult, [t_sg[pb], t_pUp[pb]], [t_hid])

        def ex_DN(ex):
            eb = ex % 2
            for jt in range(JT):
                yb = jt % 2
                for dh in range(2):
                    for fc in range(KC):
                        MM(pD[dh][:], hid[:, fc, jt * 128:(jt + 1) * 128], wdb[eb][:, fc, dh * 512:(dh + 1) * 512], fc == 0, fc == KC - 1,
                           [t_hid, t_wdb[eb]], [t_pD[dh]])
                    ACT(ysb[yb][:, dh * 512:(dh + 1) * 512], pD[dh][:], AF.Identity, [t_pD[dh], t_xgs[eb]], [t_ysb[yb]],
                        scale=xg[eb][:, jt, 512 + ex:513 + ex])
                S.dma("pool", lambda e, off_=xg[eb][:, jt, 528:529].bitcast(I32), src_=ysb[yb][:, :]: e.indirect_dma_start(
                    out=ffn_d, out_offset=bass.IndirectOffsetOnAxis(ap=off_, axis=0),
                    in_=src_, in_offset=None, compute_op=ALU.add),
                    reads=[t_ysb[yb], t_xgs[eb]], writes=[t_ffn])

        ex_T(0)
        for ex in range(NE):
            ex_GU(ex)
            if ex + 1 < NE:
                ex_T(ex + 1)
            ex_DN(ex)
        S.barrier()
    if stop_after == "X":
        return finish(nc, S, top, out_d, dbg=None)

    with contextlib.ExitStack() as ph:
        ft = [SB(ph, "ft%d" % i, [128, D]) for i in range(2)]; t_ft = [Tok(), Tok()]
        hxt = [SB(ph, "hxt%d" % i, [128, D]) for i in range(2)]; t_hxt = [Tok(), Tok()]
        ot = [SB(ph, "ot%d" % i, [128, D]) for i in range(2)]; t_ot = [Tok(), Tok()]
        junk = SB(ph, "ojunk", [128, D], BF16); t_junk = Tok()
        st4 = [SB(ph, "ost4%d" % i, [128, 4]) for i in range(2)]; t_st = [Tok(), Tok()]
        for i in range(NT):
            b = i % 2
            DMA("sp", ft[b][:], ffn_d[i * 128:(i + 1) * 128, :], [t_ffn], [t_ft[b]])
            DMA("act", hxt[b][:], hx_d[i * 128:(i + 1) * 128, :], [], [t_hxt[b]])
            ACT(junk[:], ft[b][:], AF.Square, [t_ft[b]], [t_junk, t_st[b]], accum_out=st4[b][:, 0:1])
            TS("dve", st4[b][:, 1:2], st4[b][:, 0:1], 1.0 / D, EPS, ALU.mult, ALU.add, [t_st[b]], [t_st[b]])
            ACT(st4[b][:, 2:3], st4[b][:, 1:2], AF.Sqrt, [t_st[b]], [t_st[b]])
            RECIP(st4[b][:, 3:4], st4[b][:, 2:3], [t_st[b]], [t_st[b]])
            STT("dve", ot[b][:], ft[b][:], st4[b][:, 3:4], g2row[:], ALU.mult, ALU.mult, [t_ft[b], t_st[b], t_mod], [t_ot[b]])
            TT("dve", ot[b][:], ot[b][:], hxt[b][:], ALU.add, [t_hxt[b]], [t_ot[b]])
            DMA("pool", out_d[i * 128:(i + 1) * 128, :], ot[b][:], [t_ot[b]], [])
    return finish(nc, S, top, out_d, dbg=None)


def finish(nc, S, top, out_d, dbg=None):
    if dbg is not None:
        dbg_d, src, tok, shape = dbg
        S.dma("sp", lambda e: e.dma_start(out=dbg_d[:, 0:shape[1]], in_=src[:].rearrange("p a b -> p (a b)")), reads=[tok], writes=[])
    S.barrier()
    S.emit()
    return nc


def _col(v, k):
    return np.ascontiguousarray(np.asarray(v, np.float32).reshape(k, 128).T)


def host_consts(L):
    H2 = L // 2
    k = np.arange(128)
    ang = 2.0 * np.pi * ((k[:, None] * k[None, :]) % 128) / 128.0
    scale = 1.0 / math.sqrt(L * 128.0)
    n = np.arange(H2, dtype=np.int64)
    angn = 2.0 * np.pi * ((n[:, None] * n[None, :]) % L).astype(np.float64) / L
    sgn = np.where(n % 2 == 0, 1.0, -1.0).astype(np.float32)[None, :]
    return {
        "ck": (np.cos(ang) * scale).astype(np.float32), "sk": (np.sin(ang) * scale).astype(np.float32),
        "cmat": np.cos(angn).astype(np.float32), "smat": np.sin(angn).astype(np.float32), "sgn": sgn,
    }


def make_in_maps(inp, L, CTX, nb):
    f = lambda a: np.ascontiguousarray(np.asarray(a, np.float32))
    shared = {
        "w_mod": f(inp["w_mod"][0]),
        "bmod_col": np.ascontiguousarray(f(inp["b_mod"][0])[:2048].reshape(16, 128).T),
        "bmod_row": f(inp["b_mod"][0]).reshape(1, -1),
        "nmp_col": _col(inp["norm_mix_pre"][0], 8),
        "nmpost": f(inp["norm_mix_post"][0]).reshape(1, -1),
        "nfpre": f(inp["norm_ffn_pre"][0]).reshape(1, -1),
        "nfpost": f(inp["norm_ffn_post"][0]).reshape(1, -1),
        "w_in": f(inp["w_in"][0]),
        "convw": np.ascontiguousarray(f(inp["conv_w"][0]).reshape(3, 20, 128).transpose(2, 1, 0)),
        "convb": _col(inp["conv_b"][0], 20),
        "dtb": f(inp["dt_bias"][0]).reshape(1, 48),
        "alog": f(inp["a_log"][0]).reshape(1, 48),
        "dskip": f(inp["d_skip"][0]).reshape(1, 24),
        "ssdn": f(inp["ssd_norm"][0]).reshape(1, -1),
        "w_four": f(inp["w_fourier"][0]),
        "w_ssd": f(inp["w_ssd_out"][0]),
        "bgate": np.ascontiguousarray(f(inp["b_gate"][0]).reshape(16, 128).T),
        "w_out": f(inp["w_out"][0]),
        "w_r": f(inp["w_router"][0]),
        "weg": f(inp["w_e_gate"][0]), "weu": f(inp["w_e_up"][0]), "wed": f(inp["w_e_down"][0]),
    }
    shared.update(host_consts(L))
    cctx = f(inp["c_ctx"])
    maps = []
    for b in range(nb):
        m = dict(shared)
        m["x"] = f(inp["x"][b, :L])
        m["ctx"] = f(inp["ctx"][b, :CTX])
        m["ccol"] = np.ascontiguousarray(np.stack([_col(inp["c"][b], 8), _col(cctx, 8)], axis=2))
        maps.append(m)
    return maps


_CACHE = {}


def kernel(**inputs):
    L, CTX, nb = 4096, 256, 8
    if "nc" not in _CACHE:
        _CACHE["nc"] = build_program(L, CTX)
    maps = make_in_maps(inputs, L, CTX, nb)
    res = run_bass_kernel_spmd(_CACHE["nc"], maps, core_ids=list(range(nb)))
    return np.stack([np.asarray(r["out"], np.float32) for r in res.results], axis=0)
```

```python
import contextlib
import math
import os
F_SLOT = os.environ.get('F_SLOT', '1') == '1'
F_IDD = os.environ.get('F_IDD', '1') == '1'
F_PROJ = os.environ.get('F_PROJ', '1') == '1'
F_SAME = os.environ.get('F_SAME', '1') == '1'
import numpy as np
import concourse.bass as bass
import concourse.mybir as mybir
from concourse.bass_utils import run_bass_kernel_spmd

F32 = mybir.dt.float32
BF16 = mybir.dt.bfloat16
I32 = mybir.dt.int32
AF = mybir.ActivationFunctionType
ALU = mybir.AluOpType
AX = mybir.AxisListType

D = 1024
KC = 8
GRID_W = 64
FW = 512
DI = 1536
NH = 24
NG = 4
HG = 6
HP = 64
NS = 128
NE = 16
O_Z, O_X, O_B, O_C, O_DT, O_GATE = 512, 2048, 3584, 4096, 4608, 4656
P_IN = 6704
EPS = 1e-6
RW = 544
BIG = 1.0e6


class Tok:
    __slots__ = ("w", "r")

    def __init__(self):
        self.w = None
        self.r = {}


class Sched:
    CE = ("pe", "act", "dve", "pool")
    QE = ("sp", "act", "pool")
    NPOOL = 8

    def __init__(self, nc):
        self.nc = nc
        self.prog = {e: [] for e in ("pe", "act", "dve", "pool", "sp")}
        self.cnt = {e: 0 for e in self.CE}
        self.seen = {e: {} for e in self.prog}
        self.dma_n = {q: 0 for q in self.QE}
        self.dma_val = {}
        self.stack = contextlib.ExitStack()
        self.sems = {}
        for e in self.CE:
            self.sems[e] = self.stack.enter_context(nc.semaphore("s_" + e))
        for q in self.QE:
            for i in range(self.NPOOL):
                k = ("dma", q, i)
                self.sems[k] = self.stack.enter_context(nc.semaphore("d_%s%d" % (q, i)))
                self.dma_val[k] = 0
        self._regs = {}

    def _wait(self, e, k, v):
        seen = self.seen[e]
        if seen.get(k, 0) >= v:
            return
        seen[k] = v
        self.prog[e].append(("wait", k, v))

    def _deps(self, e, reads, writes):
        deps = {}
        for t in reads:
            if t.w is not None:
                k, v = t.w
                if deps.get(k, 0) < v:
                    deps[k] = v
        for t in writes:
            if t.w is not None:
                k, v = t.w
                if deps.get(k, 0) < v:
                    deps[k] = v
            for k, v in t.r.items():
                if deps.get(k, 0) < v:
                    deps[k] = v
        for k, v in deps.items():
            if k == e and (e == "pe" or not F_SAME):
                continue
            self._wait(e, k, v)

    def op(self, e, fn, reads=(), writes=()):
        self._deps(e, reads, writes)
        self.cnt[e] += 1
        v = self.cnt[e]
        self.prog[e].append(("op", fn, e, 1))
        for t in writes:
            t.w = (e, v)
            t.r = {}
        for t in reads:
            if t.r.get(e, 0) < v:
                t.r[e] = v

    def dma(self, q, fn, reads=(), writes=()):
        i = self.dma_n[q] % self.NPOOL
        self.dma_n[q] += 1
        k = ("dma", q, i)
        prev = self.dma_val[k]
        if prev > 0:
            self._wait(q, k, prev)
        self._deps(q, reads, writes)
        v = prev + 16
        self.dma_val[k] = v
        self.prog[q].append(("op", fn, k, 16))
        for t in writes:
            t.w = (k, v)
            t.r = {}
        for t in reads:
            if t.r.get(k, 0) < v:
                t.r[k] = v

    def reg(self, engine, value):
        key = (id(engine), value)
        if key not in self._regs:
            self._regs[key] = engine.to_reg(value)
        return self._regs[key]

    def barrier(self):
        for e in self.prog:
            for k in self.CE:
                if k != e and self.cnt[k] > 0:
                    self._wait(e, k, self.cnt[k])
            for k, v in self.dma_val.items():
                if v > 0:
                    self._wait(e, k, v)

    def emit(self):
        nc = self.nc
        sems = self.sems
        prog = self.prog
        with nc.Block() as block:
            def run(e, engine):
                for it in prog[e]:
                    if it[0] == "wait":
                        engine.wait_ge(sems[it[1]], it[2])
                    else:
                        _, fn, k, inc = it
                        fn(engine).then_inc(sems[k], inc)

            @block.sync
            def _(engine):
                run("sp", engine)

            @block.scalar
            def _(engine):
                run("act", engine)

            @block.vector
            def _(engine):
                run("dve", engine)

            @block.gpsimd
            def _(engine):
                run("pool", engine)

            @block.tensor
            def _(engine):
                run("pe", engine)
        self.stack.close()


def build_program(L, CTX, debug=False, stop_after=None):
    NT = L // 128
    NB = L // 512
    NTC = CTX // 128
    H2 = L // 2
    NHC = H2 // 128
    PW = min(512, H2)
    NPC = H2 // PW
    CAP = 2 * L // NE
    JT = CAP // 128
    assert CAP % 128 == 0 and L % 512 == 0 and CTX <= 512 and CTX % 128 == 0

    nc = bass.Bass("TRN2", target_bir_lowering=False)
    S = Sched(nc)
    okind = "ExternalOutput" if debug else "Internal"

    def din(name, shape, dt=F32):
        return nc.dram_tensor(name, list(shape), dt, kind="ExternalInput").ap()

    def dscr(name, shape, dt=F32):
        return nc.dram_tensor(name, list(shape), dt, kind=okind).ap()

    x_d = din("x", [L, D])
    ctx_d = din("ctx", [CTX, D])
    ccol_d = din("ccol", [128, KC, 2])
    w_mod_d = din("w_mod", [D, 6 * D])
    bmod_col_d = din("bmod_col", [128, 16])
    bmod_row_d = din("bmod_row", [1, 6 * D])
    nmp_col_d = din("nmp_col", [128, KC])
    nmpost_d = din("nmpost", [1, D])
    nfpre_d = din("nfpre", [1, D])
    nfpost_d = din("nfpost", [1, D])
    w_in_d = din("w_in", [D, P_IN])
    convw_d = din("convw", [128, 20, 3])
    convb_d = din("convb", [128, 20])
    dtb_d = din("dtb", [1, 48])
    alog_d = din("alog", [1, 48])
    dskip_d = din("dskip", [1, NH])
    ssdn_d = din("ssdn", [1, DI])
    w_four_d = din("w_four", [FW, D])
    w_ssd_d = din("w_ssd", [DI, D])
    bgate_d = din("bgate", [128, 16])
    w_out_d = din("w_out", [D, D])
    w_r_d = din("w_r", [D, NE])
    weg_d = din("weg", [NE, D, D])
    weu_d = din("weu", [NE, D, D])
    wed_d = din("wed", [NE, D, D])
    ck_d = din("ck", [128, 128])
    sk_d = din("sk", [128, 128])
    cmat_d = din("cmat", [H2, H2])
    smat_d = din("smat", [H2, H2])
    sgn_d = din("sgn", [1, H2])
    out_d = nc.dram_tensor("out", [L, D], F32, kind="ExternalOutput").ap()

    hT_d = dscr("hT_d", [128, KC, L], BF16)
    hTc_d = dscr("hTc_d", [128, KC, CTX], BF16)
    y_d = dscr("y_d", [L, DI])
    sbr_d = dscr("sbr_d", [128, KC, L], BF16)
    hx_d = dscr("hx_d", [L, D])
    h2_d = dscr("h2_d", [L, 512])
    ffn_d = dscr("ffn_d", [L, D])
    xg_d = dscr("xg_d", [NE * CAP, RW])
    dbg_d = dscr("dbg_d", [128, 4096]) if debug else None

    def MM(out, lhsT, rhs, st, sp, r, w):
        S.op("pe", lambda e: e.matmul(out, lhsT=lhsT, rhs=rhs, start=st, stop=sp), reads=r, writes=w)

    def TR(out, in_, ident, r, w):
        S.op("pe", lambda e: e.transpose(out, in_=in_, identity=ident), reads=r, writes=w)

    def ACT(out, in_, func, r, w, **kw):
        S.op("act", lambda e: e.activation(out=out, in_=in_, func=func, **kw), reads=r, writes=w)

    def TT(eng, out, in0, in1, op, r, w):
        S.op(eng, lambda e: e.tensor_tensor(out=out, in0=in0, in1=in1, op=op), reads=r, writes=w)

    def TS(eng, out, in0, s1, s2, op0, op1, r, w):
        if s2 is None:
            S.op(eng, lambda e: e.tensor_scalar(out=out, in0=in0, scalar1=s1, scalar2=None, op0=op0), reads=r, writes=w)
        else:
            S.op(eng, lambda e: e.tensor_scalar(out=out, in0=in0, scalar1=s1, scalar2=s2, op0=op0, op1=op1), reads=r, writes=w)

    def STT(eng, out, in0, scalar, in1, op0, op1, r, w):
        S.op(eng, lambda e: e.scalar_tensor_tensor(out=out, in0=in0, scalar=scalar, in1=in1, op0=op0, op1=op1), reads=r, writes=w)

    def CP(eng, out, in_, r, w):
        if eng == "act":
            ACT(out, in_, AF.Copy, r, w)
        else:
            S.op(eng, lambda e: e.tensor_copy(out=out, in_=in_), reads=r, writes=w)

    def RED(eng, out, in_, op, r, w):
        S.op(eng, lambda e: e.tensor_reduce(out=out, in_=in_, axis=AX.X, op=op), reads=r, writes=w)

    def MSET(eng, ap, val, w):
        S.op(eng, lambda e: e.memset(ap, val), writes=w)

    def RECIP(out, in_, r, w):
        S.op("dve", lambda e: e.reciprocal(out=out, in_=in_), reads=r, writes=w)

    def TSS(out, in_, scalar, op, r, w):
        S.op("dve", lambda e: e.tensor_single_scalar(out=out, in_=in_, scalar=scalar, op=op), reads=r, writes=w)

    def DMA(q, out, in_, r, w, **kw):
        S.dma(q, lambda e: e.dma_start(out=out, in_=in_, **kw), reads=r, writes=w)

    def wview(w_ap, c0, c1):
        return w_ap.rearrange("(kc p) n -> p kc n", p=128)[:, :, c0:c1]

    top = contextlib.ExitStack()

    uniq = [0]

    def SB(stack, name, shape, dt=F32):
        uniq[0] += 1
        return stack.enter_context(nc.sbuf_tensor("sb%d_%s" % (uniq[0], name), list(shape), dt))

    def PS(stack, name, shape, dt=F32):
        uniq[0] += 1
        return stack.enter_context(nc.psum_tensor("ps%d_%s" % (uniq[0], name), list(shape), dt))

    ident_f = SB(top, "ident_f", [128, 128]); t_const = Tok()
    ident_b = SB(top, "ident_b", [128, 128], BF16)
    ones_b = SB(top, "ones_b", [128, 128], BF16)
    ones_f = SB(top, "ones_f", [128, 128])
    ltri_b = SB(top, "ltri_b", [128, 128], BF16)
    utri_b = SB(top, "utri_b", [128, 128], BF16)
    slt_f = SB(top, "slt_f", [128, 128])
    mnegf_b = SB(top, "mnegf_b", [128, 128], BF16)
    mnegb_b = SB(top, "mnegb_b", [128, 128], BF16)
    tmpc = SB(top, "tmpc", [128, 128])
    one_col = SB(top, "one_col", [128, 1])

    def tri(dst, base_val, cm, step, cmp, fill):
        MSET("pool", tmpc[:], base_val, [t_const])
        S.op("pool", lambda e, o_=tmpc[:]: e.affine_select(out=o_, in_=o_, pattern=[[step, 128]], compare_op=cmp,
                                               fill=fill, base=0, channel_multiplier=cm), reads=[t_const], writes=[t_const])
        CP("pool", dst[:], tmpc[:], [t_const], [t_const])

    tri(ident_f, 1.0, 1, -1, ALU.is_equal, 0.0)
    tri(ident_b, 1.0, 1, -1, ALU.is_equal, 0.0)
    tri(ltri_b, 1.0, -1, 1, ALU.is_ge, 0.0)
    tri(utri_b, 1.0, 1, -1, ALU.is_ge, 0.0)
    tri(slt_f, 1.0, -1, 1, ALU.is_gt, 0.0)
    tri(mnegf_b, 0.0, -1, 1, ALU.is_ge, -1.0e4)
    tri(mnegb_b, 0.0, 1, -1, ALU.is_ge, -1.0e4)
    MSET("pool", ones_b[:], 1.0, [t_const])
    MSET("pool", ones_f[:], 1.0, [t_const])
    MSET("pool", one_col[:], 1.0, [t_const])
    C = [t_const]

    a1x = SB(top, "a1x", [128, KC]); b1x = SB(top, "b1x", [128, KC])
    a1c = SB(top, "a1c", [128, KC]); b1c = SB(top, "b1c", [128, KC])
    g1row = SB(top, "g1row", [128, D]); a2row = SB(top, "a2row", [128, D])
    b2row = SB(top, "b2row", [128, D]); g2row = SB(top, "g2row", [128, D])
    t_mod = Tok()
    stackG = contextlib.ExitStack()
    dt_x = SB(stackG, "dt_x", [128, NT, 48]); da_x = SB(stackG, "da_x", [128, NT, 48])
    dt_c = SB(stackG, "dt_c", [128, NTC, 48]); da_c = SB(stackG, "da_c", [128, NTC, 48])
    t_dt = Tok()
    sc_state = SB(stackG, "sc_state", [128, 2 * NG, 384]); t_scs = [Tok() for _ in range(2 * NG)]
    dskip_bc = SB(stackG, "dskip_bc", [128, NH]); t_dsk = Tok()
    DMA("sp", dskip_bc[:], dskip_d.to_broadcast([128, NH]), [], [t_dsk])

    with contextlib.ExitStack() as ph:
        sc_col = SB(ph, "sc_col", [128, KC, 2]); t_sc = Tok()
        bmc = SB(ph, "bmc", [128, 16]); nmp = SB(ph, "nmp", [128, KC]); t_sm = Tok()
        wm = [SB(ph, "wm%d" % i, [128, KC, D]) for i in range(2)]; t_wm = [Tok(), Tok()]
        modcol = SB(ph, "modcol", [128, 16, 2]); t_mc = Tok()
        rowtmp = SB(ph, "rowtmp", [128, D]); t_rt = Tok()
        nrow = SB(ph, "nrow", [128, D]); t_nr = Tok()
        pcol = PS(ph, "pcol", [128, 32]); t_pcol = Tok()
        prow = [PS(ph, "prow%d" % i, [128, 512]) for i in range(2)]; t_prow = [Tok(), Tok()]
        DMA("sp", sc_col[:], ccol_d, [], [t_sc])
        DMA("sp", bmc[:], bmod_col_d, [], [t_sm])
        DMA("sp", nmp[:], nmp_col_d, [], [t_sm])
        ACT(sc_col[:], sc_col[:], AF.Silu, [t_sc], [t_sc])
        for m in range(6):
            b = m % 2
            DMA("sp" if m % 2 == 0 else "act", wm[b][:], wview(w_mod_d, m * D, (m + 1) * D), [], [t_wm[b]])
            if m < 2:
                for dc in range(8):
                    for kc in range(KC):
                        MM(pcol[:, (m * 8 + dc) * 2:(m * 8 + dc) * 2 + 2], wm[b][:, kc, dc * 128:(dc + 1) * 128],
                           sc_col[:, kc, :], kc == 0, kc == KC - 1, [t_wm[b], t_sc], [t_pcol])
                if m == 1:
                    TT("dve", modcol[:], pcol[:].rearrange("p (a b) -> p a b", b=2),
                       bmc[:].unsqueeze(2).to_broadcast([128, 16, 2]), ALU.add, [t_pcol, t_sm], [t_mc])
            else:
                dst = {2: g1row, 3: b2row, 4: a2row, 5: g2row}[m]
                DMA("sp", rowtmp[:], bmod_row_d[:, m * D:(m + 1) * D].to_broadcast([128, D]), [], [t_rt])
                for hf in range(2):
                    for kc in range(KC):
                        MM(prow[hf][:], sc_col[:, kc, 0:1].to_broadcast([128, 128]), wm[b][:, kc, hf * 512:(hf + 1) * 512],
                           kc == 0, kc == KC - 1, [t_wm[b], t_sc], [t_prow[hf]])
                    TT("dve", dst[:, hf * 512:(hf + 1) * 512], prow[hf][:], rowtmp[:, hf * 512:(hf + 1) * 512], ALU.add,
                       [t_prow[hf], t_rt], [t_mod])
                if m == 2:
                    DMA("act", nrow[:], nmpost_d.to_broadcast([128, D]), [], [t_nr])
                    TT("dve", g1row[:], g1row[:], nrow[:], ALU.mult, [t_nr, t_mod], [t_mod])
                elif m == 4:
                    DMA("act", nrow[:], nfpre_d.to_broadcast([128, D]), [], [t_nr])
                    STT("dve", a2row[:], a2row[:], 1.0, nrow[:], ALU.add, ALU.mult, [t_nr, t_mod], [t_mod])
                elif m == 5:
                    DMA("act", nrow[:], nfpost_d.to_broadcast([128, D]), [], [t_nr])
                    TT("dve", g2row[:], g2row[:], nrow[:], ALU.mult, [t_nr, t_mod], [t_mod])
        STT("dve", a1x[:], modcol[:, 8:16, 0], 1.0, nmp[:], ALU.add, ALU.mult, [t_mc, t_sm], [t_mod])
        STT("dve", a1c[:], modcol[:, 8:16, 1], 1.0, nmp[:], ALU.add, ALU.mult, [t_mc, t_sm], [t_mod])
        CP("dve", b1x[:], modcol[:, 0:8, 0], [t_mc], [t_mod])
        CP("dve", b1c[:], modcol[:, 0:8, 1], [t_mc], [t_mod])
        S.barrier()
    if stop_after == "M":
        if debug:
            mod_dbg = nc.dram_tensor("mod_dbg", [128, 4, D], F32, kind="ExternalOutput").ap()
            for qi, tt_ in enumerate((g1row, a2row, b2row, g2row)):
                DMA("sp", mod_dbg[:, qi, :], tt_[:], [t_mod], [])
            col_dbg = nc.dram_tensor("col_dbg", [128, 4, KC], F32, kind="ExternalOutput").ap()
            for qi, tt_ in enumerate((a1x, b1x, a1c, b1c)):
                DMA("sp", col_dbg[:, qi, :], tt_[:], [t_mod], [])
        return finish(nc, S, top, out_d, dbg=None)

    with contextlib.ExitStack() as ph:
        xt = [SB(ph, "xt%d" % i, [128, D]) for i in range(2)]; t_xt = [Tok(), Tok()]
        junk = SB(ph, "junk", [128, D], BF16); t_junk = Tok()
        st4 = [SB(ph, "st4%d" % i, [128, 4]) for i in range(2)]; t_st = [Tok(), Tok()]
        xn = [SB(ph, "xn%d" % i, [128, D], BF16) for i in range(2)]; t_xn = [Tok(), Tok()]
        tmpf = SB(ph, "tmpf", [128, KC, 128]); t_tmpf = Tok()
        hTt = [SB(ph, "hTt%d" % i, [128, KC, 128], BF16) for i in range(2)]; t_hTt = [Tok(), Tok()]
        wdt = SB(ph, "wdt", [128, KC, 48], BF16); t_wdt = Tok()
        dtb_bc = SB(ph, "dtb_bc", [128, 48]); aneg_bc = SB(ph, "aneg_bc", [128, 48]); t_ab = Tok()
        pT = [PS(ph, "pT%d" % i, [128, D], BF16) for i in range(2)]; t_pT = [Tok(), Tok()]
        pdt = [PS(ph, "pdt%d" % i, [128, 64]) for i in range(2)]; t_pdt = [Tok(), Tok()]
        DMA("pool", wdt[:], wview(w_in_d, O_DT, O_DT + 48), [], [t_wdt])
        DMA("sp", dtb_bc[:], dtb_d.to_broadcast([128, 48]), [], [t_ab])
        DMA("sp", aneg_bc[:], alog_d.to_broadcast([128, 48]), [], [t_ab])
        ACT(aneg_bc[:], aneg_bc[:], AF.Exp, [t_ab], [t_ab])
        TS("dve", aneg_bc[:], aneg_bc[:], -1.0, None, ALU.mult, None, [t_ab], [t_ab])

        def norm_T(src, ntile, acol, bcol, dst, dtraw):
            for i in range(ntile):
                b = i % 2
                DMA("sp", xt[b][:], src[i * 128:(i + 1) * 128, :], [], [t_xt[b]])
                ACT(junk[:], xt[b][:], AF.Square, [t_xt[b]], [t_junk, t_st[b]], accum_out=st4[b][:, 0:1])
                TS("dve", st4[b][:, 1:2], st4[b][:, 0:1], 1.0 / D, EPS, ALU.mult, ALU.add, [t_st[b]], [t_st[b]])
                ACT(st4[b][:, 2:3], st4[b][:, 1:2], AF.Sqrt, [t_st[b]], [t_st[b]])
                RECIP(st4[b][:, 3:4], st4[b][:, 2:3], [t_st[b]], [t_st[b]])
                ACT(xn[b][:], xt[b][:], AF.Identity, [t_xt[b], t_st[b]], [t_xn[b]], scale=st4[b][:, 3:4])
                for kc in range(KC):
                    TR(pT[b][:, kc * 128:(kc + 1) * 128], xn[b][:, kc * 128:(kc + 1) * 128], ident_b[:], [t_xn[b]] + C, [t_pT[b]])
                TT("dve", tmpf[:], pT[b][:].rearrange("p (k t) -> p k t", k=KC), acol[:].unsqueeze(2).to_broadcast([128, KC, 128]),
                   ALU.mult, [t_pT[b], t_mod], [t_tmpf])
                TT("dve", hTt[b][:], tmpf[:], bcol[:].unsqueeze(2).to_broadcast([128, KC, 128]), ALU.add, [t_tmpf, t_mod], [t_hTt[b]])
                DMA("act", dst[:, :, i * 128:(i + 1) * 128], hTt[b][:], [t_hTt[b]], [])
                for kc in range(KC):
                    MM(pdt[b][:, 0:48], hTt[b][:, kc, :], wdt[:, kc, :], kc == 0, kc == KC - 1, [t_hTt[b], t_wdt], [t_pdt[b]])
                CP("act", dtraw[:, i, :], pdt[b][:, 0:48], [t_pdt[b]], [t_dt])

        def dt_post(dtt, dat, ntile, stack):
            va = SB(stack, "va%d" % ntile, [128, ntile, 48]); vb = SB(stack, "vb%d" % ntile, [128, ntile, 48]); t_v = Tok()
            TT("dve", dtt[:], dtt[:], dtb_bc[:].unsqueeze(1).to_broadcast([128, ntile, 48]), ALU.add, [t_dt, t_ab], [t_dt])
            TS("dve", va[:], dtt[:], 30.0, None, ALU.min, None, [t_dt], [t_v])
            ACT(va[:], va[:], AF.Exp, [t_v], [t_v])
            TS("dve", va[:], va[:], 1.0, None, ALU.add, None, [t_v], [t_v])
            ACT(vb[:], va[:], AF.Ln, [t_v], [t_v])
            TT("dve", dtt[:], dtt[:], vb[:], ALU.max, [t_dt, t_v], [t_dt])
            TT("dve", dat[:], dtt[:], aneg_bc[:].unsqueeze(1).to_broadcast([128, ntile, 48]), ALU.mult, [t_dt, t_ab], [t_dt])

        norm_T(ctx_d, NTC, a1c, b1c, hTc_d, dt_c)
        if stop_after == "N1":
            return finish(nc, S, top, out_d, dbg=None)
        dt_post(dt_c, da_c, NTC, ph)
        if stop_after == "N2":
            return finish(nc, S, top, out_d, dbg=None)
        norm_T(x_d, NT, a1x, b1x, hT_d, dt_x)
        dt_post(dt_x, da_x, NT, ph)
        S.barrier()
    if stop_after == "N":
        return finish(nc, S, top, out_d, dbg=None)

    zero_y = None
    with contextlib.ExitStack() as ph:
        wg = SB(ph, "wg", [128, KC, 640], BF16); t_wg = Tok()
        cw = SB(ph, "cw", [128, 20, 3]); cb = SB(ph, "cb", [128, 20]); t_cw = Tok()
        DMA("sp", cw[:], convw_d, [], [t_cw])
        DMA("sp", cb[:], convb_d, [], [t_cw])
        hTb = [SB(ph, "hTb%d" % i, [128, KC, 512], BF16) for i in range(2)]; t_hTb = [Tok(), Tok()]
        acc = [SB(ph, "acc%d" % i, [128, 512]) for i in range(2)]; t_acc = [Tok(), Tok()]
        xTb = [SB(ph, "xTb%d" % i, [128, 512], BF16) for i in range(2)]; t_xTb = [Tok(), Tok()]
        x_tm = SB(ph, "x_tm", [128, NT, 384], BF16); t_xtm = [Tok() for _ in range(NT)]
        b_tm = SB(ph, "b_tm", [128, NT, 128], BF16); t_btm = [Tok() for _ in range(NT)]
        bT = SB(ph, "bT", [128, L], BF16); t_bT = [Tok() for _ in range(NB)]
        cT = SB(ph, "cT", [128, L], BF16); t_cT = [Tok() for _ in range(NB)]
        identD = SB(ph, "identD", [128, NH, 128], BF16); t_idD = Tok()
        for hh in range(NH):
            TS("dve", identD[:, hh, :], ident_f[:], dskip_bc[:, hh:hh + 1], None, ALU.mult, None, [t_dsk] + C, [t_idD])

        class DirBufs:
            pass
        dirs = []
        for dd in range(2):
            o = DirBufs()
            sfx = "_d%d" % dd
            o.dahi = SB(ph, "dahi" + sfx, [128, NT, HG], BF16); o.dalo = SB(ph, "dalo" + sfx, [128, NT, HG], BF16); o.t_da = Tok()
            o.datmp = SB(ph, "datmp" + sfx, [128, NT, HG])
            o.nacum = SB(ph, "nacum" + sfx, [128, NT, HG]); o.eacum = SB(ph, "eacum" + sfx, [128, NT, HG])
            o.wst = SB(ph, "wst" + sfx, [128, NT, HG]); o.cdec = SB(ph, "cdec" + sfx, [128, NT, HG]); o.t_hd = Tok()
            o.xdt = [SB(ph, "xdt%d" % i + sfx, [128, 384], BF16) for i in range(2)]; o.t_xdt = [Tok(), Tok()]
            o.xw = [SB(ph, "xw%d" % i + sfx, [128, 384], BF16) for i in range(2)]; o.t_xw = [Tok(), Tok()]
            o.cbT = [SB(ph, "cbT%d" % i + sfx, [128, 128], BF16) for i in range(2)]; o.t_cbT = [Tok(), Tok()]
            o.eT = [SB(ph, "eT%d" % i + sfx, [128, HG, 128], BF16) for i in range(2)]; o.t_eT = [Tok(), Tok()]
            o.mT = [SB(ph, "mT%d" % i + sfx, [128, HG, 128], BF16) for i in range(2)]; o.t_mT = [Tok(), Tok()]
            o.ytmp = [SB(ph, "ytmp%d" % i + sfx, [128, 384]) for i in range(2)]; o.t_ytmp = [Tok(), Tok()]
            o.yc = [SB(ph, "yc%d" % i + sfx, [128, 384]) for i in range(2)]; o.t_yc = [Tok(), Tok()]
            o.stsb = [SB(ph, "stsb%d" % i + sfx, [128, 384]) for i in range(2)]; o.t_stsb = [Tok(), Tok()]
            o.s_f = SB(ph, "s_f" + sfx, [128, 384]); o.s_b16 = SB(ph, "s_b16" + sfx, [128, 384], BF16); o.t_s = Tok(); o.t_sb = Tok()
            dirs.append(o)
        pP = [PS(ph, "pP%d" % i, [128, 512]) for i in range(2)]; t_pP = [Tok(), Tok()]
        pX = PS(ph, "pX", [128, 4, 128], BF16); t_pX = Tok()
        pR = [PS(ph, "pR%d" % i, [128, 4, 128]) for i in range(2)]; t_pR = [Tok() for _ in range(HG)]
        pY = PS(ph, "pY", [128, 512]); t_pY = Tok()
        pO = PS(ph, "pO", [128, 512]); t_pO = Tok()
        pS = PS(ph, "pS", [128, 512]); t_pS = Tok(); t_pCB = Tok()

        def ssd_group(g, is_ctx):
            ntile = NTC if is_ctx else NT
            LL = CTX if is_ctx else L
            bw = min(512, LL)
            nblk = LL // bw
            rl = CTX if is_ctx else GRID_W
            nr = bw // rl
            src = hTc_d if is_ctx else hT_d
            dtt = dt_c if is_ctx else dt_x
            dat = da_c if is_ctx else da_x
            DMA("pool", wg[:, :, 0:384], wview(w_in_d, O_X + g * 384, O_X + (g + 1) * 384), [], [t_wg])
            DMA("pool", wg[:, :, 384:512], wview(w_in_d, O_B + g * 128, O_B + (g + 1) * 128), [], [t_wg])
            DMA("pool", wg[:, :, 512:640], wview(w_in_d, O_C + g * 128, O_C + (g + 1) * 128), [], [t_wg])
            chtile = [(O_X - O_X) // 128 + g * 3 + 0, g * 3 + 1, g * 3 + 2, 12 + g, 16 + g]
            nsub = bw // 128
            jobs = [(nb, ct) for nb in range(nblk) for ct in range(5)]

            def proj_mm(k):
                nb, ct = jobs[k]
                hb = nb % 2
                pb = k % 2
                if ct == 0:
                    DMA("sp", hTb[hb][:, :, 0:bw], src[:, :, nb * bw:(nb + 1) * bw], [], [t_hTb[hb]])
                for kc in range(KC):
                    MM(pP[pb][:, 0:bw], wg[:, kc, ct * 128:(ct + 1) * 128], hTb[hb][:, kc, 0:bw], kc == 0, kc == KC - 1,
                       [t_wg, t_hTb[hb]], [t_pP[pb]])

            def proj_post(k):
                nb, ct = jobs[k]
                pb = k % 2
                cti = chtile[ct]
                pv = pP[pb][:, 0:bw].rearrange("p (r c) -> p r c", c=rl)
                av = acc[pb][:, 0:bw].rearrange("p (r c) -> p r c", c=rl)
                TS("dve", acc[pb][:, 0:bw], pP[pb][:, 0:bw], cw[:, cti, 1:2], cb[:, cti:cti + 1], ALU.mult, ALU.add,
                   [t_pP[pb], t_cw], [t_acc[pb]])
                STT("dve", av[:, :, 1:rl], pv[:, :, 0:rl - 1], cw[:, cti, 0:1], av[:, :, 1:rl], ALU.mult, ALU.add,
                    [t_pP[pb], t_cw, t_acc[pb]], [t_acc[pb]])
                STT("dve", av[:, :, 0:rl - 1], pv[:, :, 1:rl], cw[:, cti, 2:3], av[:, :, 0:rl - 1], ALU.mult, ALU.add,
                    [t_pP[pb], t_cw, t_acc[pb]], [t_acc[pb]])
                if ct < 3:
                    ACT(xTb[pb][:, 0:bw], acc[pb][:, 0:bw], AF.Silu, [t_acc[pb]], [t_xTb[pb]])
                elif ct == 3:
                    ACT(bT[:, nb * bw:(nb + 1) * bw], acc[pb][:, 0:bw], AF.Silu, [t_acc[pb]], [t_bT[nb]])
                else:
                    ACT(cT[:, nb * bw:(nb + 1) * bw], acc[pb][:, 0:bw], AF.Silu, [t_acc[pb]], [t_cT[nb]])

            def proj_tr(k):
                nb, ct = jobs[k]
                pb = k % 2
                if ct < 3:
                    for j in range(nsub):
                        TR(pX[:, j, :], xTb[pb][:, j * 128:(j + 1) * 128], ident_b[:], [t_xTb[pb]] + C, [t_pX])
                    tl = [t_xtm[nb * nsub + j] for j in range(nsub)]
                    CP("act", x_tm[:, nb * nsub:(nb + 1) * nsub, ct * 128:(ct + 1) * 128], pX[:, 0:nsub, :], [t_pX], tl)
                elif ct == 3:
                    for j in range(nsub):
                        TR(pX[:, j, :], bT[:, nb * bw + j * 128:nb * bw + (j + 1) * 128], ident_b[:], [t_bT[nb]] + C, [t_pX])
                    tl = [t_btm[nb * nsub + j] for j in range(nsub)]
                    CP("act", b_tm[:, nb * nsub:(nb + 1) * nsub, :], pX[:, 0:nsub, :], [t_pX], tl)

            if F_PROJ:
                for k in range(len(jobs) + 1):
                    if k < len(jobs):
                        proj_mm(k)
                        proj_post(k)
                    if k >= 1:
                        proj_tr(k - 1)
            else:
                for k in range(len(jobs)):
                    proj_mm(k)
                    proj_post(k)
                    proj_tr(k)
            nn = ntile * HG
            for d in range(2):
                o = dirs[d]
                o.hsl = slice(d * NH + g * HG, d * NH + (g + 1) * HG)
                o.trib = ltri_b if d == 0 else utri_b
                o.mneg = mnegf_b if d == 0 else mnegb_b
                o.order = list(range(ntile)) if d == 0 else list(range(ntile - 1, -1, -1))
                CP("dve", o.dahi[:, 0:ntile, :], dat[:, :, o.hsl], [t_dt], [o.t_da])
                TT("dve", o.datmp[:, 0:ntile, :], dat[:, :, o.hsl], o.dahi[:, 0:ntile, :], ALU.subtract, [t_dt, o.t_da], [o.t_da])
                CP("dve", o.dalo[:, 0:ntile, :], o.datmp[:, 0:ntile, :], [o.t_da], [o.t_da])
                hi2 = o.dahi[:, 0:ntile, :].rearrange("p c h -> p (c h)")
                lo2 = o.dalo[:, 0:ntile, :].rearrange("p c h -> p (c h)")
                MM(pP[0][:, 0:nn], o.trib[:], hi2, True, False, [o.t_da] + C, [t_pP[0]])
                MM(pP[0][:, 0:nn], o.trib[:], lo2, False, True, [o.t_da] + C, [t_pP[0]])
                MM(pP[1][:, 0:nn], ones_b[:], hi2, True, False, [o.t_da] + C, [t_pP[1]])
                MM(pP[1][:, 0:nn], ones_b[:], lo2, False, True, [o.t_da] + C, [t_pP[1]])
                na2 = o.nacum[:, 0:ntile, :].rearrange("p c h -> p (c h)")
                ea2 = o.eacum[:, 0:ntile, :].rearrange("p c h -> p (c h)")
                ws2 = o.wst[:, 0:ntile, :].rearrange("p c h -> p (c h)")
                cd2 = o.cdec[:, 0:ntile, :].rearrange("p c h -> p (c h)")
                TS("dve", na2, pP[0][:, 0:nn], -1.0, None, ALU.mult, None, [t_pP[0]], [o.t_hd])
                ACT(ea2, pP[0][:, 0:nn], AF.Exp, [t_pP[0]], [o.t_hd])
                TT("dve", ws2, pP[1][:, 0:nn], na2, ALU.add, [t_pP[1], o.t_hd], [o.t_hd])
                ACT(ws2, ws2, AF.Exp, [o.t_hd], [o.t_hd])
                TT("dve", o.wst[:, 0:ntile, :], o.wst[:, 0:ntile, :], dtt[:, :, o.hsl], ALU.mult, [o.t_hd, t_dt], [o.t_hd])
                ACT(cd2, pP[1][:, 0:nn], AF.Exp, [t_pP[1]], [o.t_hd])
                if is_ctx:
                    MSET("dve", o.s_f[:], 0.0, [o.t_s])
                else:
                    CP("dve", o.s_f[:], sc_state[:, d * NG + g, :], [t_scs[d * NG + g]], [o.t_s])
                CP("act", o.s_b16[:], o.s_f[:], [o.t_s], [o.t_sb])

            rcount = [0]

            def stage_pre(d, it):
                o = dirs[d]
                c = o.order[it]
                b = it % 2
                xv = x_tm[:, c, :].rearrange("p (h q) -> p h q", q=HP)
                if not is_ctx:
                    TT("dve", o.xdt[b][:].rearrange("p (h q) -> p h q", q=HP), xv, dtt[:, c, o.hsl].unsqueeze(2).to_broadcast([128, HG, HP]),
                       ALU.mult, [t_xtm[c], t_dt], [o.t_xdt[b]])
                TT("dve", o.xw[b][:].rearrange("p (h q) -> p h q", q=HP), xv, o.wst[:, c, :].unsqueeze(2).to_broadcast([128, HG, HP]),
                   ALU.mult, [t_xtm[c], o.t_hd], [o.t_xw[b]])

            def stage_a(d, it):
                o = dirs[d]
                c = o.order[it]
                b = it % 2
                nb = c // nsub
                xv = x_tm[:, c, :].rearrange("p (h q) -> p h q", q=HP)
                if not is_ctx:
                    for h in range(HG):
                        rb = rcount[0] % 4
                        rcount[0] += 1
                        if rb < 2:
                            prv = pR[rb][:, 0, :]; tpr = t_pR[rb]
                        else:
                            prv = pP[rb - 2][:, 0:128]; tpr = t_pP[rb - 2]
                        MM(prv, o.dahi[:, c, h:h + 1].to_broadcast([128, 128]), o.trib[:], True, False, [o.t_da] + C, [tpr])
                        MM(prv, o.dalo[:, c, h:h + 1].to_broadcast([128, 128]), o.trib[:], False, False, [o.t_da] + C, [tpr])
                        MM(prv, ident_b[:], o.mneg[:], False, True, C, [tpr])
                        ACT(o.eT[b][:, h, :], prv, AF.Exp, [tpr, o.t_hd], [o.t_eT[b]], bias=o.nacum[:, c, h:h + 1])
                    csl = slice(c * 128, (c + 1) * 128)
                    MM(pS[:, 384:512], bT[:, csl], cT[:, csl], True, True, [t_bT[nb], t_cT[nb]], [t_pCB])
                    CP("act", o.cbT[b][:], pS[:, 384:512], [t_pCB], [o.t_cbT[b]])
                MM(pS[:, 0:384], b_tm[:, c, :], o.xw[b][:], True, True, [t_btm[c], o.t_xw[b]], [t_pS])
                CP("act", o.stsb[b][:], pS[:, 0:384], [t_pS], [o.t_stsb[b]])
                if not is_ctx:
                    TT("dve", o.mT[b][:], o.eT[b][:], o.cbT[b][:].unsqueeze(1).to_broadcast([128, HG, 128]), ALU.mult,
                       [o.t_eT[b], o.t_cbT[b]], [o.t_mT[b]])

            def stage_b(d, it):
                o = dirs[d]
                c = o.order[it]
                b = it % 2
                nb = c // nsub
                if not is_ctx:
                    csl = slice(c * 128, (c + 1) * 128)
                    for h in range(HG):
                        hh = g * HG + h
                        last = (d != 0) or not F_IDD
                        MM(pY[:, h * HP:(h + 1) * HP], o.mT[b][:, h, :], o.xdt[b][:, h * HP:(h + 1) * HP], True, last,
                           [o.t_mT[b], o.t_xdt[b]], [t_pY])
                        if d == 0 and F_IDD:
                            MM(pY[:, h * HP:(h + 1) * HP], identD[:, hh, :], x_tm[:, c, h * HP:(h + 1) * HP], False, True,
                               [t_idD, t_xtm[c]], [t_pY])
                    MM(pO[:, 0:384], cT[:, csl], o.s_b16[:], True, True, [t_cT[nb], o.t_sb], [t_pO])
                    TT("dve", o.ytmp[b][:].rearrange("p (h q) -> p h q", q=HP), pO[:, 0:384].rearrange("p (h q) -> p h q", q=HP),
                       o.eacum[:, c, :].unsqueeze(2).to_broadcast([128, HG, HP]), ALU.mult, [t_pO, o.t_hd], [o.t_ytmp[b]])
                    TT("dve", o.yc[b][:], o.ytmp[b][:], pY[:, 0:384], ALU.add, [o.t_ytmp[b], t_pY], [o.t_yc[b]])
                    if d == 0 and not F_IDD:
                        xv = x_tm[:, c, :].rearrange("p (h q) -> p h q", q=HP)
                        TT("dve", o.ytmp[b][:].rearrange("p (h q) -> p h q", q=HP), xv,
                           dskip_bc[:, g * HG:(g + 1) * HG].unsqueeze(2).to_broadcast([128, HG, HP]), ALU.mult,
                           [t_xtm[c], t_dsk, o.t_yc[b]], [o.t_ytmp[b]])
                        TT("dve", o.yc[b][:], o.yc[b][:], o.ytmp[b][:], ALU.add, [o.t_ytmp[b]], [o.t_yc[b]])
                    ydst = y_d[c * 128:(c + 1) * 128, g * 384:(g + 1) * 384]
                    if not y_written[g][c]:
                        y_written[g][c] = True
                        DMA("sp", ydst, o.yc[b][:], [o.t_yc[b]], [t_yd[g][c]])
                    else:
                        S.dma("pool", lambda e, ydst=ydst, src_=o.yc[b][:]: e.dma_start(out=ydst, in_=src_, accum_op=ALU.add),
                              reads=[o.t_yc[b]], writes=[t_yd[g][c]])
                TT("dve", o.s_f[:].rearrange("p (h q) -> p h q", q=HP), o.s_f[:].rearrange("p (h q) -> p h q", q=HP),
                   o.cdec[:, c, :].unsqueeze(2).to_broadcast([128, HG, HP]), ALU.mult, [o.t_hd, o.t_s], [o.t_s])
                TT("dve", o.s_f[:], o.s_f[:], o.stsb[b][:], ALU.add, [o.t_stsb[b], o.t_s], [o.t_s])
                CP("act", o.s_b16[:], o.s_f[:], [o.t_s], [o.t_sb])

            for d in range(2):
                stage_pre(d, 0)
            for it in range(ntile + 1):
                for d in range(2):
                    if it < ntile:
                        stage_a(d, it)
                    if it >= 1:
                        stage_b(d, it - 1)
                    if it + 1 < ntile:
                        stage_pre(d, it + 1)
            if is_ctx:
                for d in range(2):
                    CP("dve", sc_state[:, d * NG + g, :], dirs[d].s_f[:], [dirs[d].t_s], [t_scs[d * NG + g]])

        y_written = [[False] * NT for _ in range(NG)]
        t_yd = [[Tok() for _ in range(NT)] for _ in range(NG)]
        for g in range(NG):
            ssd_group(g, True)
        for g in range(NG):
            ssd_group(g, False)
        S.barrier()
    if debug:
        sc_dbg = nc.dram_tensor("sc_dbg", [128, 2 * NG, 384], F32, kind="ExternalOutput").ap()
        DMA("sp", sc_dbg, sc_state[:], t_scs, [])
        S.barrier()
    if stop_after == "G":
        return finish(nc, S, top, out_d, dbg=None)
    stackG.close()

    fT = SB(top, "fT", [128, 4, L], BF16); t_fT = Tok()
    with contextlib.ExitStack() as ph:
        ck = SB(ph, "ckb", [128, 128], BF16); sk = SB(ph, "skb", [128, 128], BF16); t_ck = Tok()
        sgn = SB(ph, "sgnb", [1, H2], BF16)
        ue = SB(ph, "ue", [128, 4, H2], BF16); uo = SB(ph, "uo", [128, 4, H2], BF16); t_ue = Tok()
        u2048 = SB(ph, "u2048", [1, 512], BF16); t_u2 = Tok()
        vv = SB(ph, "vv", [128, 4, 4]); vb16 = SB(ph, "vb16", [128, 4], BF16); t_vv = Tok()
        pU = [PS(ph, "pU%d" % i, [128, 512]) for i in range(2)]; t_pU = [Tok(), Tok()]
        pFc = [PS(ph, "pFc%d" % i, [128, 512]) for i in range(2)]; t_pFc = [Tok(), Tok()]
        pFs = [PS(ph, "pFs%d" % i, [128, 512]) for i in range(2)]; t_pFs = [Tok(), Tok()]
        ph2 = contextlib.ExitStack()
        wu = SB(ph2, "wu", [128, KC, 512], BF16); t_wu = Tok()
        hTb = [SB(ph2, "uhTb%d" % i, [128, KC, 512], BF16) for i in range(2)]; t_hTb = [Tok(), Tok()]
        uT = SB(ph2, "uT", [128, 4, L], BF16); t_uT = Tok()
        DMA("pool", wu[:], wview(w_in_d, 0, 512), [], [t_wu])
        DMA("pool", ck[:], ck_d, [], [t_ck])
        DMA("pool", sk[:], sk_d, [], [t_ck])
        DMA("pool", sgn[:], sgn_d, [], [t_ck])
        for nb in range(NB):
            hb = nb % 2
            DMA("sp", hTb[hb][:], hT_d[:, :, nb * 512:(nb + 1) * 512], [], [t_hTb[hb]])
            for g in range(4):
                pb = g % 2
                for kc in range(KC):
                    MM(pU[pb][:], wu[:, kc, g * 128:(g + 1) * 128], hTb[hb][:, kc, :], kc == 0, kc == KC - 1, [t_wu, t_hTb[hb]], [t_pU[pb]])
                CP("act", uT[:, g, nb * 512:(nb + 1) * 512], pU[pb][:], [t_pU[pb]], [t_uT])
        for g in range(4):
            rev = uT[:, g, H2 + 1:L][:, ::-1]
            TT("dve", ue[:, g, 1:H2], uT[:, g, 1:H2], rev, ALU.add, [t_uT], [t_ue])
            TT("dve", uo[:, g, 1:H2], uT[:, g, 1:H2], rev, ALU.subtract, [t_uT], [t_ue])
            CP("dve", ue[:, g, 0:1], uT[:, g, 0:1], [t_uT], [t_ue])
            MSET("dve", uo[:, g, 0:1], 0.0, [t_ue])
            uv = uT[:, g, :].rearrange("p (n two) -> p two n", two=2)
            RED("dve", vv[:, g, 0:2], uv, ALU.add, [t_uT], [t_vv])
            TT("dve", vv[:, g, 2:3], vv[:, g, 0:1], vv[:, g, 1:2], ALU.subtract, [t_vv], [t_vv])
            CP("dve", vb16[:, g:g + 1], vv[:, g, 2:3], [t_vv], [t_vv])
        for g in range(4):
            MM(pU[0][0:1, g * 128:(g + 1) * 128], uT[:, g, H2:H2 + 1], ck[:], True, True, [t_uT, t_ck], [t_pU[0]])
        CP("act", u2048[:], pU[0][0:1, :], [t_pU[0]], [t_u2])
        for g in range(4):
            MM(pU[1][:, g:g + 1], ck[:], vb16[:, g:g + 1], True, True, [t_vv, t_ck], [t_pU[1]])
        for g in range(4):
            CP("act", fT[:, g, H2:H2 + 1], pU[1][:, g:g + 1], [t_pU[1]], [t_fT])
        S.barrier()
        ph2.close()
        e_tm = SB(ph, "e_tm", [128, NHC, 512], BF16); o_tm = SB(ph, "o_tm", [128, NHC, 512], BF16); t_eo = Tok()
        cblk = [SB(ph, "cblk%d" % i, [128, NHC, PW], BF16) for i in range(1)] * 2; t_cblk = [Tok()] * 2
        sblk = [SB(ph, "sblk%d" % i, [128, NHC, PW], BF16) for i in range(1)] * 2; t_sblk = [Tok()] * 2
        sfc = [SB(ph, "sfc%d" % i, [128, PW]) for i in range(2)]; sfs = [SB(ph, "sfs%d" % i, [128, PW]) for i in range(2)]
        t_sf = [Tok(), Tok()]
        for n in range(NHC):
            for (srcT, mat, dst, pb) in ((ue, ck, e_tm, 0), (uo, sk, o_tm, 1)):
                for g in range(4):
                    MM(pU[pb][:, g * 128:(g + 1) * 128], srcT[:, g, n * 128:(n + 1) * 128], mat[:], True, True, [t_ue, t_ck], [t_pU[pb]])
                CP("act", dst[:, n, :], pU[pb][:], [t_pU[pb]], [t_eo])
        for pc in range(NPC):
            cbf = pc % 2
            DMA("pool", cblk[cbf][:], cmat_d.rearrange("(n p) q -> p n q", p=128)[:, :, pc * PW:(pc + 1) * PW], [], [t_cblk[cbf]])
            DMA("pool", sblk[cbf][:], smat_d.rearrange("(n p) q -> p n q", p=128)[:, :, pc * PW:(pc + 1) * PW], [], [t_sblk[cbf]])
            for g in range(4):
                pb = g % 2
                for n in range(NHC):
                    MM(pFc[pb][:, 0:PW], e_tm[:, n, g * 128:(g + 1) * 128], cblk[cbf][:, n, :], n == 0, False, [t_eo, t_cblk[cbf]], [t_pFc[pb]])
                MM(pFc[pb][:, 0:PW], u2048[0:1, g * 128:(g + 1) * 128], sgn[0:1, pc * PW:(pc + 1) * PW], False, True, [t_u2, t_ck], [t_pFc[pb]])
                for n in range(NHC):
                    MM(pFs[pb][:, 0:PW], o_tm[:, n, g * 128:(g + 1) * 128], sblk[cbf][:, n, :], n == 0, n == NHC - 1, [t_eo, t_sblk[cbf]], [t_pFs[pb]])
                CP("act", sfc[pb][:], pFc[pb][:, 0:PW], [t_pFc[pb]], [t_sf[pb]])
                CP("act", sfs[pb][:], pFs[pb][:, 0:PW], [t_pFs[pb]], [t_sf[pb]])
                p0 = pc * PW
                TT("dve", fT[:, g, p0:p0 + PW], sfc[pb][:], sfs[pb][:], ALU.subtract, [t_sf[pb]], [t_fT])
                lo = 1 if pc == 0 else 0
                TT("dve", fT[:, g, L - p0 - PW + 1:L - p0 - lo + 1], sfc[pb][:, lo:PW][:, ::-1], sfs[pb][:, lo:PW][:, ::-1], ALU.add,
                   [t_sf[pb]], [t_fT])
        S.barrier()
    if debug:
        fT_dbg = nc.dram_tensor("fT_dbg", [128, 4, L], BF16, kind="ExternalOutput").ap()
        DMA("sp", fT_dbg, fT[:], [t_fT], [])
    if stop_after == "U":
        return finish(nc, S, top, out_d, dbg=None)

    with contextlib.ExitStack() as ph:
        wz = SB(ph, "wz", [128, KC, DI], BF16); t_wz = Tok()
        wss = SB(ph, "wss", [128, 12, D], BF16); t_wss = Tok()
        ssdn = SB(ph, "ssdn", [128, DI]); t_ssdn = Tok()
        hTb = [SB(ph, "zhTb%d" % i, [128, KC, 512], BF16) for i in range(2)]; t_hTb = [Tok(), Tok()]
        zs = [SB(ph, "zs%d" % i, [128, DI]) for i in range(2)]; t_zs = [Tok(), Tok()]
        yt = [SB(ph, "yt%d" % i, [128, DI]) for i in range(2)]; t_yt = [Tok(), Tok()]
        junk = SB(ph, "zjunk", [128, DI], BF16); t_junk = Tok()
        st4 = [SB(ph, "zst4%d" % i, [128, 4]) for i in range(2)]; t_st = [Tok(), Tok()]
        yzn = [SB(ph, "yzn%d" % i, [128, DI], BF16) for i in range(2)]; t_yzn = [Tok(), Tok()]
        yzT = [SB(ph, "yzT0", [128, 12, 512], BF16)] * 2; t_yzT = [Tok()] * 2
        sbrT = [SB(ph, "sbrT0", [128, KC, 512], BF16)] * 2; t_sbrT = [Tok()] * 2
        pZ = [PS(ph, "pZ%d" % i, [128, 512]) for i in range(3)]; t_pZ = [Tok(), Tok(), Tok()]
        pTz = PS(ph, "pTz", [128, 2048], BF16); t_pTz = Tok()
        pSb = [PS(ph, "pSb%d" % i, [128, 512]) for i in range(2)]; t_pSb = [Tok(), Tok()]
        DMA("pool", wz[:], wview(w_in_d, O_Z, O_Z + DI), [], [t_wz])
        DMA("pool", wss[:], wview(w_ssd_d, 0, D), [], [t_wss])
        DMA("sp", ssdn[:], ssdn_d.to_broadcast([128, DI]), [], [t_ssdn])
        def z1_stage1(i):
            nb, j = divmod(i, 4)
            hb = nb % 2
            b = i % 2
            if j == 0:
                DMA("sp", hTb[hb][:], hT_d[:, :, nb * 512:(nb + 1) * 512], [], [t_hTb[hb]])
            DMA("act", yt[b][:], y_d[i * 128:(i + 1) * 128, :], [], [t_yt[b]])
            for zc in range(3):
                for kc in range(KC):
                    MM(pZ[zc][:], hTb[hb][:, kc, j * 128:(j + 1) * 128], wz[:, kc, zc * 512:(zc + 1) * 512], kc == 0, kc == KC - 1,
                       [t_hTb[hb], t_wz], [t_pZ[zc]])
                ACT(zs[b][:, zc * 512:(zc + 1) * 512], pZ[zc][:], AF.Silu, [t_pZ[zc]], [t_zs[b]])
            TT("dve", zs[b][:], zs[b][:], yt[b][:], ALU.mult, [t_zs[b], t_yt[b]], [t_zs[b]])
            ACT(junk[:], zs[b][:], AF.Square, [t_zs[b]], [t_junk, t_st[b]], accum_out=st4[b][:, 0:1])
            TS("dve", st4[b][:, 1:2], st4[b][:, 0:1], 1.0 / DI, EPS, ALU.mult, ALU.add, [t_st[b]], [t_st[b]])
            ACT(st4[b][:, 2:3], st4[b][:, 1:2], AF.Sqrt, [t_st[b]], [t_st[b]])
            RECIP(st4[b][:, 3:4], st4[b][:, 2:3], [t_st[b]], [t_st[b]])
            STT("dve", yzn[b][:], zs[b][:], st4[b][:, 3:4], ssdn[:], ALU.mult, ALU.mult, [t_zs[b], t_st[b], t_ssdn], [t_yzn[b]])

        def z1_stage2(i):
            nb, j = divmod(i, 4)
            hb = nb % 2
            b = i % 2
            for ctile in range(12):
                TR(pTz[:, ctile * 128:(ctile + 1) * 128], yzn[b][:, ctile * 128:(ctile + 1) * 128], ident_b[:], [t_yzn[b]] + C, [t_pTz])
            CP("act", yzT[hb][:, :, j * 128:(j + 1) * 128], pTz[:, 0:1536].rearrange("p (c t) -> p c t", c=12), [t_pTz], [t_yzT[hb]])
            if j == 3:
                for dc in range(KC):
                    pb = dc % 2
                    for ctile in range(12):
                        MM(pSb[pb][:], wss[:, ctile, dc * 128:(dc + 1) * 128], yzT[hb][:, ctile, :], ctile == 0, ctile == 11,
                           [t_wss, t_yzT[hb]], [t_pSb[pb]])
                    CP("act", sbrT[hb][:, dc, :], pSb[pb][:], [t_pSb[pb]], [t_sbrT[hb]])
                DMA("sp", sbr_d[:, :, nb * 512:(nb + 1) * 512], sbrT[hb][:], [t_sbrT[hb]], [])

        for i in range(NT + 1):
            if i < NT:
                z1_stage1(i)
            if i >= 1:
                z1_stage2(i - 1)
        S.barrier()
    if stop_after == "Z1":
        return finish(nc, S, top, out_d, dbg=None)

    logits = SB(top, "logits", [128, NT, NE]); t_lg = Tok()
    with contextlib.ExitStack() as ph:
        wgt = SB(ph, "wgt", [128, KC, 2 * D], BF16); t_wgt = Tok()
        wfo = SB(ph, "wfo", [128, 4, D], BF16); t_wfo = Tok()
        wo = SB(ph, "wo", [128, KC, D], BF16); t_wo = Tok()
        wr = SB(ph, "wr", [128, KC, NE]); t_wr = Tok()
        bg = SB(ph, "bg", [128, 16]); t_bg = Tok()
        hTb = [SB(ph, "yhTb%d" % i, [128, KC, 512], BF16) for i in range(2)]; t_hTb = [Tok(), Tok()]
        sbrT = [SB(ph, "ysbrT0", [128, KC, 512], BF16)] * 2; t_sbrT = [Tok()] * 2
        gT = SB(ph, "gT", [128, 16, 512], BF16); t_gT = Tok()
        m1 = [SB(ph, "m1%d" % i, [128, 512]) for i in range(2)]; t_m1 = [Tok(), Tok()]
        m2 = [SB(ph, "m20", [128, 512])] * 2; t_m2 = [Tok()] * 2
        mrg = [SB(ph, "mrg0", [128, KC, 512], BF16)] * 2; t_mrg = [Tok()] * 2
        mix = [SB(ph, "mix%d" % i, [128, D]) for i in range(2)]; t_mix = [Tok(), Tok()]
        xt = [SB(ph, "zxt%d" % i, [128, D]) for i in range(2)]; t_xt = [Tok(), Tok()]
        hx = [SB(ph, "hx%d" % i, [128, D]) for i in range(2)]; t_hx = [Tok(), Tok()]
        h2f = [SB(ph, "h2f%d" % i, [128, D]) for i in range(2)]; t_h2f = [Tok(), Tok()]
        h2b = [SB(ph, "h2b0", [128, 512])] * 2; t_h2b = [Tok()] * 2
        h2T = SB(ph, "h2T", [128, KC, 128]); t_h2T = Tok()
        junk = SB(ph, "yjunk", [128, D], BF16); t_junk = Tok()
        st8 = [SB(ph, "st8%d" % i, [128, 8]) for i in range(2)]; t_st = [Tok(), Tok()]
        pA = [PS(ph, "pA%d" % i, [128, 512]) for i in range(4)]; t_pA = [Tok() for _ in range(4)]
        pH = PS(ph, "pH", [128, KC, 128]); t_pH = Tok()
        pL = PS(ph, "pL", [128, 512]); t_pL = Tok()
        DMA("pool", wgt[:], wview(w_in_d, O_GATE, O_GATE + 2 * D), [], [t_wgt])
        DMA("pool", wfo[:], wview(w_four_d, 0, D), [], [t_wfo])
        DMA("pool", wo[:], wview(w_out_d, 0, D), [], [t_wo])
        DMA("sp", wr[:], wview(w_r_d, 0, NE), [], [t_wr])
        DMA("sp", bg[:], bgate_d, [], [t_bg])
        for nb in range(NB):
            hb = nb % 2
            DMA("sp", hTb[hb][:], hT_d[:, :, nb * 512:(nb + 1) * 512], [], [t_hTb[hb]])
            DMA("act", sbrT[hb][:], sbr_d[:, :, nb * 512:(nb + 1) * 512], [], [t_sbrT[hb]])
            for gi in range(16):
                pb = gi % 2
                for kc in range(KC):
                    MM(pA[pb][:], wgt[:, kc, gi * 128:(gi + 1) * 128], hTb[hb][:, kc, :], kc == 0, kc == KC - 1, [t_wgt, t_hTb[hb]], [t_pA[pb]])
                ACT(gT[:, gi, :], pA[pb][:], AF.Sigmoid, [t_pA[pb], t_bg], [t_gT], bias=bg[:, gi:gi + 1])
            for dc in range(KC):
                pb = 2 + dc % 2
                b = dc % 2
                for g in range(4):
                    MM(pA[pb][:], wfo[:, g, dc * 128:(dc + 1) * 128], fT[:, g, nb * 512:(nb + 1) * 512], g == 0, g == 3, [t_wfo, t_fT], [t_pA[pb]])
                TT("dve", m1[b][:], pA[pb][:], gT[:, dc, :], ALU.mult, [t_pA[pb], t_gT], [t_m1[b]])
                TT("dve", m2[b][:], sbrT[hb][:, dc, :], gT[:, 8 + dc, :], ALU.mult, [t_sbrT[hb], t_gT], [t_m2[b]])
                TT("dve", mrg[hb][:, dc, :], m1[b][:], m2[b][:], ALU.add, [t_m1[b], t_m2[b]], [t_mrg[hb]])
            def z2_mix(j, nb=nb, hb=hb):
                i = nb * 4 + j
                b = i % 2
                DMA("sp", xt[b][:], x_d[i * 128:(i + 1) * 128, :], [], [t_xt[b]])
                for dh in range(2):
                    pb = dh
                    for kc in range(KC):
                        MM(pA[pb][:], mrg[hb][:, kc, j * 128:(j + 1) * 128], wo[:, kc, dh * 512:(dh + 1) * 512], kc == 0, kc == KC - 1,
                           [t_mrg[hb], t_wo], [t_pA[pb]])
                    CP("act", mix[b][:, dh * 512:(dh + 1) * 512], pA[pb][:], [t_pA[pb]], [t_mix[b]])
                ACT(junk[:], mix[b][:], AF.Square, [t_mix[b]], [t_junk, t_st[b]], accum_out=st8[b][:, 0:1])
                TS("dve", st8[b][:, 1:2], st8[b][:, 0:1], 1.0 / D, EPS, ALU.mult, ALU.add, [t_st[b]], [t_st[b]])
                ACT(st8[b][:, 2:3], st8[b][:, 1:2], AF.Sqrt, [t_st[b]], [t_st[b]])
                RECIP(st8[b][:, 3:4], st8[b][:, 2:3], [t_st[b]], [t_st[b]])
                STT("dve", mix[b][:], mix[b][:], st8[b][:, 3:4], g1row[:], ALU.mult, ALU.mult, [t_mix[b], t_st[b], t_mod], [t_mix[b]])
                TT("dve", hx[b][:], mix[b][:], xt[b][:], ALU.add, [t_mix[b], t_xt[b]], [t_hx[b]])
                DMA("act", hx_d[i * 128:(i + 1) * 128, :], hx[b][:], [t_hx[b]], [])
                ACT(junk[:], hx[b][:], AF.Square, [t_hx[b]], [t_junk, t_st[b]], accum_out=st8[b][:, 4:5])
                TS("dve", st8[b][:, 5:6], st8[b][:, 4:5], 1.0 / D, EPS, ALU.mult, ALU.add, [t_st[b]], [t_st[b]])
                ACT(st8[b][:, 6:7], st8[b][:, 5:6], AF.Sqrt, [t_st[b]], [t_st[b]])
                RECIP(st8[b][:, 7:8], st8[b][:, 6:7], [t_st[b]], [t_st[b]])
                STT("dve", h2f[b][:], hx[b][:], st8[b][:, 7:8], a2row[:], ALU.mult, ALU.mult, [t_hx[b], t_st[b], t_mod], [t_h2f[b]])
                TT("dve", h2f[b][:], h2f[b][:], b2row[:], ALU.add, [t_mod], [t_h2f[b]])
                CP("act", h2b[b][:].bitcast(BF16), h2f[b][:], [t_h2f[b]], [t_h2b[b]])
                DMA("act", h2_d[i * 128:(i + 1) * 128, :], h2b[b][:], [t_h2b[b]], [])

            def z2_router(j, nb=nb):
                i = nb * 4 + j
                b = i % 2
                for kc in range(KC):
                    TR(pH[:, kc, :], h2f[b][:, kc * 128:(kc + 1) * 128], ident_f[:], [t_h2f[b]] + C, [t_pH])
                CP("act", h2T[:], pH[:], [t_pH], [t_h2T])
                for kc in range(KC):
                    MM(pL[:, 0:NE], h2T[:, kc, :], wr[:, kc, :], kc == 0, kc == KC - 1, [t_h2T, t_wr], [t_pL])
                CP("dve", logits[:, i, :], pL[:, 0:NE], [t_pL], [t_lg])

            for j in range(5):
                if j < 4:
                    z2_mix(j)
                if j >= 1:
                    z2_router(j - 1)
        S.barrier()
    if stop_after == "Z2":
        return finish(nc, S, top, out_d, dbg=None)

    aff = SB(top, "aff", [128, NT, NE]); t_aff = Tok()
    t_xg = Tok(); t_ffn = Tok()
    offi = SB(top, "offi", [128, NT * NE], I32); t_offi = Tok()
    tokid = SB(top, "tokid", [128, NT], I32); t_tokid = Tok()
    S.op("pool", lambda e, o_=tokid[:]: e.iota(o_, pattern=[[128, NT]], base=0, channel_multiplier=1), writes=[t_tokid])
    with contextlib.ExitStack() as ph:
        mx = SB(ph, "mx", [128, NT]); t_mx = Tok()
        msk = SB(ph, "msk", [128, NT, NE]); t_msk = Tok()
        part = SB(ph, "part", [128, NE]); t_part = Tok()
        lo_t = SB(ph, "lo_t", [128, NE]); mid = SB(ph, "mid", [128, NE]); sel = SB(ph, "sel", [128, NE]); t_lo = Tok()
        posf = SB(ph, "posf", [128, NT, NE]); t_pos = Tok()
        base = SB(ph, "base", [128, NT, NE]); t_base = Tok()
        eoff = SB(ph, "eoff", [128, NE]); t_eoff = Tok()
        pC_ = PS(ph, "pC_", [128, 512]); t_pC = Tok()
        pW = PS(ph, "pW", [128, 512]); t_pW = Tok()
        pTt = PS(ph, "pTt", [128, 512]); t_pTt = Tok()
        zt = SB(ph, "zt", [128, D]); t_zt = Tok()
        MSET("pool", zt[:], 0.0, [t_zt])
        for i in range(NT):
            DMA("act", ffn_d[i * 128:(i + 1) * 128, :], zt[:], [t_zt], [t_ffn])
        RED("dve", mx[:], logits[:], ALU.max, [t_lg], [t_mx])
        TT("dve", aff[:], logits[:], mx[:].unsqueeze(2).to_broadcast([128, NT, NE]), ALU.subtract, [t_lg, t_mx], [t_aff])
        ACT(aff[:], aff[:], AF.Exp, [t_aff], [t_aff])
        RED("dve", mx[:], aff[:], ALU.add, [t_aff], [t_mx])
        RECIP(mx[:], mx[:], [t_mx], [t_mx])
        TT("dve", aff[:], aff[:], mx[:].unsqueeze(2).to_broadcast([128, NT, NE]), ALU.mult, [t_mx, t_aff], [t_aff])
        MSET("dve", lo_t[:], 0.0, [t_lo])
        w = 0.5
        for it in range(30):
            TS("dve", mid[:], lo_t[:], w, None, ALU.add, None, [t_lo], [t_lo])
            TT("dve", msk[:], aff[:], mid[:].unsqueeze(1).to_broadcast([128, NT, NE]), ALU.is_ge, [t_aff, t_lo], [t_msk])
            RED("dve", part[:], msk[:].rearrange("p i e -> p e i"), ALU.add, [t_msk], [t_part])
            MM(pC_[:, 0:NE], ones_f[:], part[:], True, True, [t_part] + C, [t_pC])
            TSS(sel[:], pC_[:, 0:NE], CAP - 0.5, ALU.is_ge, [t_pC], [t_lo])
            STT("dve", lo_t[:], sel[:], w, lo_t[:], ALU.mult, ALU.add, [t_lo], [t_lo])
            w *= 0.5
        TT("dve", msk[:], aff[:], lo_t[:].unsqueeze(1).to_broadcast([128, NT, NE]), ALU.is_ge, [t_aff, t_lo], [t_msk])
        m2d = msk[:].rearrange("p i e -> p (i e)")
        nn = NT * NE
        MM(pW[:, 0:nn], slt_f[:], m2d, True, True, [t_msk] + C, [t_pW])
        MM(pTt[:, 0:nn], ones_f[:], m2d, True, True, [t_msk] + C, [t_pTt])
        MSET("dve", base[:, 0, :], 0.0, [t_base])
        for i in range(1, NT):
            TT("dve", base[:, i, :], base[:, i - 1, :], pTt[:, (i - 1) * NE:i * NE], ALU.add, [t_pTt, t_base], [t_base])
        TT("dve", posf[:].rearrange("p i e -> p (i e)"), base[:].rearrange("p i e -> p (i e)"), pW[:, 0:nn], ALU.add, [t_base, t_pW], [t_pos])
        TSS(base[:], posf[:], CAP - 0.5, ALU.is_lt, [t_pos], [t_base])
        TT("dve", msk[:], msk[:], base[:], ALU.mult, [t_base, t_msk], [t_msk])
        S.op("pool", lambda e, o_=eoff[:].bitcast(I32): e.iota(o_, pattern=[[CAP, NE]], base=0, channel_multiplier=0), writes=[t_eoff])
        CP("dve", base[:, 0, :], eoff[:].bitcast(I32), [t_eoff, t_base], [t_base])
        TS("dve", base[:, 0, :], base[:, 0, :], -BIG, None, ALU.add, None, [t_base], [t_base])
        TT("dve", posf[:], posf[:], base[:, 0, :].unsqueeze(1).to_broadcast([128, NT, NE]), ALU.add, [t_base, t_pos], [t_pos])
        TT("dve", posf[:], posf[:], msk[:], ALU.mult, [t_msk, t_pos], [t_pos])
        TS("dve", posf[:], posf[:], BIG, None, ALU.add, None, [t_pos], [t_pos])
        CP("dve", offi[:], posf[:].rearrange("p i e -> p (i e)"), [t_pos], [t_offi])
        S.barrier()

    with contextlib.ExitStack() as ph:
        phs = contextlib.ExitStack()
        rowt = [SB(phs, "rowt%d" % i, [128, RW]) for i in range(2)]; t_rowt = [Tok(), Tok()]
        for b in range(2):
            MSET("pool", rowt[b][:], 0.0, [t_rowt[b]])
        for i in range(NT):
            b = i % 2
            DMA("sp", rowt[b][:, 0:512], h2_d[i * 128:(i + 1) * 128, :], [], [t_rowt[b]])
            CP("dve", rowt[b][:, 512:528], aff[:, i, :], [t_aff], [t_rowt[b]])
            CP("dve", rowt[b][:, 528:529].bitcast(I32), tokid[:, i:i + 1], [t_tokid], [t_rowt[b]])
            for ex in range(NE):
                col = i * NE + ex
                S.dma("pool", lambda e, src_=rowt[b][:, :], off_=offi[:, col:col + 1]: e.indirect_dma_start(
                    out=xg_d, out_offset=bass.IndirectOffsetOnAxis(ap=off_, axis=0),
                    in_=src_, in_offset=None, bounds_check=S.reg(e, NE * CAP - 1), oob_is_err=False),
                    reads=[t_rowt[b], t_offi], writes=[])
        S.barrier()
        phs.close()
        wgb = [SB(ph, "wgb%d" % i, [128, KC, D], BF16) for i in range(2)]; t_wgb = [Tok(), Tok()]
        wub = [SB(ph, "wub%d" % i, [128, KC, D], BF16) for i in range(2)]; t_wub = [Tok(), Tok()]
        wdb = [SB(ph, "wdb%d" % i, [128, KC, D], BF16) for i in range(2)]; t_wdb = [Tok(), Tok()]
        xg = [SB(ph, "xg%d" % i, [128, JT, RW]) for i in range(2)]; t_xgs = [Tok(), Tok()]
        xgT = [SB(ph, "xgT%d" % i, [128, KC, CAP], BF16) for i in range(2)]; t_xgT = [Tok(), Tok()]
        sg = [SB(ph, "sg%d" % i, [128, CAP]) for i in range(2)]; t_sg = [Tok(), Tok()]
        hid = SB(ph, "hid", [128, KC, CAP], BF16); t_hid = Tok()
        ysb = [SB(ph, "ysb%d" % i, [128, D]) for i in range(2)]; t_ysb = [Tok(), Tok()]
        pXt = PS(ph, "pXt", [128, D], BF16); t_pXt = Tok()
        pG = [PS(ph, "pG%d" % i, [128, 512]) for i in range(2)]; t_pG = [Tok(), Tok()]
        pUp = [PS(ph, "pUp%d" % i, [128, 512]) for i in range(2)]; t_pUp = [Tok(), Tok()]
        pD = [PS(ph, "pD%d" % i, [128, 512]) for i in range(2)]; t_pD = [Tok(), Tok()]
        ncol = (CAP + 511) // 512

        def ex_T(ex):
            eb = ex % 2
            DMA("pool", wgb[eb][:], weg_d[ex].rearrange("(kc p) n -> p kc n", p=128), [], [t_wgb[eb]])
            DMA("pool", wub[eb][:], weu_d[ex].rearrange("(kc p) n -> p kc n", p=128), [], [t_wub[eb]])
            DMA("pool", wdb[eb][:], wed_d[ex].rearrange("(kc p) n -> p kc n", p=128), [], [t_wdb[eb]])
            DMA("sp", xg[eb][:], xg_d[ex * CAP:(ex + 1) * CAP, :].rearrange("(j p) w -> p j w", p=128), [], [t_xgs[eb]])
            for jt in range(JT):
                xrow = xg[eb][:, jt, 0:512].bitcast(BF16)
                for kc in range(KC):
                    TR(pXt[:, kc * 128:(kc + 1) * 128], xrow[:, kc * 128:(kc + 1) * 128], ident_b[:], [t_xgs[eb]] + C, [t_pXt])
                CP("act", xgT[eb][:, :, jt * 128:(jt + 1) * 128], pXt[:].rearrange("p (k t) -> p k t", k=KC), [t_pXt], [t_xgT[eb]])

        def ex_GU(ex):
            eb = ex % 2
            for fc in range(KC):
                for cc in range(ncol):
                    c0 = cc * 512
                    c1 = min(CAP, c0 + 512)
                    pb = (fc * ncol + cc) % 2
                    for kc in range(KC):
                        MM(pG[pb][:, 0:c1 - c0], wgb[eb][:, kc, fc * 128:(fc + 1) * 128], xgT[eb][:, kc, c0:c1], kc == 0, kc == KC - 1,
                           [t_wgb[eb], t_xgT[eb]], [t_pG[pb]])
                    for kc in range(KC):
                        MM(pUp[pb][:, 0:c1 - c0], wub[eb][:, kc, fc * 128:(fc + 1) * 128], xgT[eb][:, kc, c0:c1], kc == 0, kc == KC - 1,
                           [t_wub[eb], t_xgT[eb]], [t_pUp[pb]])
                    ACT(sg[pb][:, 0:c1 - c0], pG[pb][:, 0:c1 - c0], AF.Silu, [t_pG[pb]], [t_sg[pb]])
                    TT("dve", hid[:, fc, c0:c1], sg[pb][:, 0:c1 - c0], pUp[pb][:, 0:c1 - c0], ALU.mult, [t_sg[pb], t_pUp[pb]], [t_hid])

        def ex_DN(ex):
            eb = ex % 2
            for jt in range(JT):
                yb = jt % 2
                for dh in range(2):
                    for fc in range(KC):
                        MM(pD[dh][:], hid[:, fc, jt * 128:(jt + 1) * 128], wdb[eb][:, fc, dh * 512:(dh + 1) * 512], fc == 0, fc == KC - 1,
                           [t_hid, t_wdb[eb]], [t_pD[dh]])
                    ACT(ysb[yb][:, dh * 512:(dh + 1) * 512], pD[dh][:], AF.Identity, [t_pD[dh], t_xgs[eb]], [t_ysb[yb]],
                        scale=xg[eb][:, jt, 512 + ex:513 + ex])
                S.dma("pool", lambda e, off_=xg[eb][:, jt, 528:529].bitcast(I32), src_=ysb[yb][:, :]: e.indirect_dma_start(
                    out=ffn_d, out_offset=bass.IndirectOffsetOnAxis(ap=off_, axis=0),
                    in_=src_, in_offset=None, compute_op=ALU.add),
                    reads=[t_ysb[yb], t_xgs[eb]], writes=[t_ffn])

        ex_T(0)
        for ex in range(NE):
            ex_GU(ex)
            if ex + 1 < NE:
                ex_T(ex + 1)
            ex_DN(ex)
        S.barrier()
    if stop_after == "X":
        return finish(nc, S, top, out_d, dbg=None)

    with contextlib.ExitStack() as ph:
        ft = [SB(ph, "ft%d" % i, [128, D]) for i in range(2)]; t_ft = [Tok(), Tok()]
        hxt = [SB(ph, "hxt%d" % i, [128, D]) for i in range(2)]; t_hxt = [Tok(), Tok()]
        ot = [SB(ph, "ot%d" % i, [128, D]) for i in range(2)]; t_ot = [Tok(), Tok()]
        junk = SB(ph, "ojunk", [128, D], BF16); t_junk = Tok()
        st4 = [SB(ph, "ost4%d" % i, [128, 4]) for i in range(2)]; t_st = [Tok(), Tok()]
        for i in range(NT):
            b = i % 2
            DMA("sp", ft[b][:], ffn_d[i * 128:(i + 1) * 128, :], [t_ffn], [t_ft[b]])
            DMA("act", hxt[b][:], hx_d[i * 128:(i + 1) * 128, :], [], [t_hxt[b]])
            ACT(junk[:], ft[b][:], AF.Square, [t_ft[b]], [t_junk, t_st[b]], accum_out=st4[b][:, 0:1])
            TS("dve", st4[b][:, 1:2], st4[b][:, 0:1], 1.0 / D, EPS, ALU.mult, ALU.add, [t_st[b]], [t_st[b]])
            ACT(st4[b][:, 2:3], st4[b][:, 1:2], AF.Sqrt, [t_st[b]], [t_st[b]])
            RECIP(st4[b][:, 3:4], st4[b][:, 2:3], [t_st[b]], [t_st[b]])
            STT("dve", ot[b][:], ft[b][:], st4[b][:, 3:4], g2row[:], ALU.mult, ALU.mult, [t_ft[b], t_st[b], t_mod], [t_ot[b]])
            TT("dve", ot[b][:], ot[b][:], hxt[b][:], ALU.add, [t_hxt[b]], [t_ot[b]])
            DMA("pool", out_d[i * 128:(i + 1) * 128, :], ot[b][:], [t_ot[b]], [])
    return finish(nc, S, top, out_d, dbg=None)


def finish(nc, S, top, out_d, dbg=None):
    if dbg is not None:
        dbg_d, src, tok, shape = dbg
        S.dma("sp", lambda e: e.dma_start(out=dbg_d[:, 0:shape[1]], in_=src[:].rearrange("p a b -> p (a b)")), reads=[tok], writes=[])
    S.barrier()
    S.emit()
    return nc


def _col(v, k):
    return np.ascontiguousarray(np.asarray(v, np.float32).reshape(k, 128).T)


def host_consts(L):
    H2 = L // 2
    k = np.arange(128)
    ang = 2.0 * np.pi * ((k[:, None] * k[None, :]) % 128) / 128.0
    scale = 1.0 / math.sqrt(L * 128.0)
    n = np.arange(H2, dtype=np.int64)
    angn = 2.0 * np.pi * ((n[:, None] * n[None, :]) % L).astype(np.float64) / L
    sgn = np.where(n % 2 == 0, 1.0, -1.0).astype(np.float32)[None, :]
    return {
        "ck": (np.cos(ang) * scale).astype(np.float32), "sk": (np.sin(ang) * scale).astype(np.float32),
        "cmat": np.cos(angn).astype(np.float32), "smat": np.sin(angn).astype(np.float32), "sgn": sgn,
    }


def make_in_maps(inp, L, CTX, nb):
    f = lambda a: np.ascontiguousarray(np.asarray(a, np.float32))
    shared = {
        "w_mod": f(inp["w_mod"][0]),
        "bmod_col": np.ascontiguousarray(f(inp["b_mod"][0])[:2048].reshape(16, 128).T),
        "bmod_row": f(inp["b_mod"][0]).reshape(1, -1),
        "nmp_col": _col(inp["norm_mix_pre"][0], 8),
        "nmpost": f(inp["norm_mix_post"][0]).reshape(1, -1),
        "nfpre": f(inp["norm_ffn_pre"][0]).reshape(1, -1),
        "nfpost": f(inp["norm_ffn_post"][0]).reshape(1, -1),
        "w_in": f(inp["w_in"][0]),
        "convw": np.ascontiguousarray(f(inp["conv_w"][0]).reshape(3, 20, 128).transpose(2, 1, 0)),
        "convb": _col(inp["conv_b"][0], 20),
        "dtb": f(inp["dt_bias"][0]).reshape(1, 48),
        "alog": f(inp["a_log"][0]).reshape(1, 48),
        "dskip": f(inp["d_skip"][0]).reshape(1, 24),
        "ssdn": f(inp["ssd_norm"][0]).reshape(1, -1),
        "w_four": f(inp["w_fourier"][0]),
        "w_ssd": f(inp["w_ssd_out"][0]),
        "bgate": np.ascontiguousarray(f(inp["b_gate"][0]).reshape(16, 128).T),
        "w_out": f(inp["w_out"][0]),
        "w_r": f(inp["w_router"][0]),
        "weg": f(inp["w_e_gate"][0]), "weu": f(inp["w_e_up"][0]), "wed": f(inp["w_e_down"][0]),
    }
    shared.update(host_consts(L))
    cctx = f(inp["c_ctx"])
    maps = []
    for b in range(nb):
        m = dict(shared)
        m["x"] = f(inp["x"][b, :L])
        m["ctx"] = f(inp["ctx"][b, :CTX])
        m["ccol"] = np.ascontiguousarray(np.stack([_col(inp["c"][b], 8), _col(cctx, 8)], axis=2))
        maps.append(m)
    return maps


_CACHE = {}


def kernel(**inputs):
    L, CTX, nb = 4096, 256, 8
    if "nc" not in _CACHE:
        _CACHE["nc"] = build_program(L, CTX)
    maps = make_in_maps(inputs, L, CTX, nb)
    res = run_bass_kernel_spmd(_CACHE["nc"], maps, core_ids=list(range(nb)))
    return np.stack([np.asarray(r["out"], np.float32) for r in res.results], axis=0)
```
